# Optimizing a Trainium2 kernel written in Bass

```python
import jax
import jax.numpy as jnp
from jax import lax

D_MODEL = 4096
BATCH = 16
SEQ = 256
DEPTH = 2
DEC_BATCH = 8
DEC_SEQ = 4096
PAST_LEN = 512

GRID_W = 64
N_EVEN = (DEPTH + 1) // 2
N_ODD = DEPTH // 2
N_MOD = 6
EPS = 1e-6
NEG_INF = -1e30
ROPE_BASE = 10000.0
ADA_SCALE = 0.3
RET_HEADS = 8
RET_DK = 256
RET_DV = 256
RET_CHUNK = 128
RET_QK_W = RET_HEADS * RET_DK
RET_W = RET_HEADS * RET_DV
ATT_HEADS = 16
ATT_KV_HEADS = 4
ATT_GROUP = ATT_HEADS // ATT_KV_HEADS
ATT_HEAD_DIM = 128
ATT_W = ATT_HEADS * ATT_HEAD_DIM
ATT_KV_W = ATT_KV_HEADS * ATT_HEAD_DIM
ATT_SCALE = ATT_HEAD_DIM ** -0.5
WINDOW = 128
ATT_BLOCK = 128
MIX_W = RET_W + ATT_W
IN_EVEN = 2 * RET_QK_W + 2 * RET_W + ATT_W + 2 * ATT_KV_W
EVEN_SPLITS = [RET_QK_W, 2 * RET_QK_W, 2 * RET_QK_W + RET_W, 2 * RET_QK_W + 2 * RET_W,
               2 * RET_QK_W + 2 * RET_W + ATT_W, 2 * RET_QK_W + 2 * RET_W + ATT_W + ATT_KV_W]
FFN_DIM = 11008
GMLP_W = D_MODEL
GMLP_GROUPS = 16
GMLP_CHUNK = 128
N_EXPERTS = 8
TOP_K = 2
EXPERT_DIM = 2048

kernel_name = 'hybrid_diffusion_retention_window_gmlp_moe'


def rms_norm(x, g):
    xf = x.astype(jnp.float32)
    y = xf * lax.rsqrt(jnp.mean(xf * xf, axis=-1, keepdims=True) + EPS)
    return (y * g.astype(jnp.float32)).astype(x.dtype)


def head_rms(x, dtype):
    xf = x.astype(jnp.float32)
    return (xf * lax.rsqrt(jnp.mean(xf * xf, axis=-1, keepdims=True) + EPS)).astype(dtype)


def layer_norm(x, g):
    xf = x.astype(jnp.float32)
    xc = xf - jnp.mean(xf, axis=-1, keepdims=True)
    y = xc * lax.rsqrt(jnp.mean(xc * xc, axis=-1, keepdims=True) + EPS)
    return (y * g.astype(jnp.float32)).astype(x.dtype)


def adaln(cond, w, b):
    m = jax.nn.silu(cond.astype(jnp.float32)).astype(w.dtype) @ w + b
    m = m.reshape(cond.shape[0], 1, N_MOD, D_MODEL)
    return [m[:, :, i] for i in range(N_MOD)]


def modulate(x, g, shift, scale):
    return rms_norm(x, g) * (1 + scale) + shift


def rope_2d(x):
    seq, dim = x.shape[1], x.shape[-1]
    rows = seq // GRID_W
    row = jnp.repeat(jnp.arange(rows), GRID_W)
    col = jnp.tile(jnp.arange(GRID_W), rows)
    half = dim // 2
    nf = half // 2
    inv = ROPE_BASE ** (-jnp.arange(nf, dtype=jnp.float32) / nf)

    def rot(xp, pos):
        ang = pos.astype(jnp.float32)[:, None] * inv[None, :]
        cos = jnp.cos(ang)[None, :, None, :]
        sin = jnp.sin(ang)[None, :, None, :]
        x1, x2 = xp[..., :nf], xp[..., nf:]
        return jnp.concatenate([x1 * cos - x2 * sin, x1 * sin + x2 * cos], axis=-1)

    xf = x.astype(jnp.float32)
    return jnp.concatenate([rot(xf[..., :half], row), rot(xf[..., half:], col)], axis=-1).astype(x.dtype)


def retention_dir(q, k, v, log_g, s0, inclusive):
    b, seq, nh, _ = q.shape
    dv = v.shape[-1]
    n = seq // RET_CHUNK

    def chunks(t):
        return t.astype(jnp.float32).reshape(b, n, RET_CHUNK, nh, t.shape[-1]).transpose(1, 0, 3, 2, 4)

    idx = jnp.arange(RET_CHUNK, dtype=jnp.float32)
    diff = idx[:, None] - idx[None, :]
    keep = diff >= 0 if inclusive else diff > 0
    intra = jnp.where(keep[None], jnp.exp(jnp.maximum(diff, 0.0)[None] * log_g[:, None, None]), 0.0)
    q_dec = jnp.exp((idx + 1.0)[None, :] * log_g[:, None])[..., None]
    k_dec = jnp.exp((RET_CHUNK - 1.0 - idx)[None, :] * log_g[:, None])[..., None]
    c_dec = jnp.exp(RET_CHUNK * log_g)[:, None, None]

    def step(s, inp):
        qc, kc, vc = inp
        att = jnp.einsum('bhid,bhjd->bhij', qc, kc) * intra
        o = jnp.einsum('bhij,bhjv->bhiv', att, vc) + jnp.einsum('bhid,bhdv->bhiv', qc * q_dec, s)
        s = s * c_dec + jnp.einsum('bhjd,bhjv->bhdv', kc * k_dec, vc)
        return s, o

    s_fin, o = lax.scan(step, s0.astype(jnp.float32), (chunks(q), chunks(k), chunks(v)))
    o = o.transpose(1, 0, 3, 2, 4).reshape(b, seq, nh, dv)
    return o, s_fin


def bidir_retention(q, k, v, log_g, s0_f, s0_b):
    o_f, s_f = retention_dir(q, k, v, log_g[0], s0_f, True)
    o_b, s_b = retention_dir(q[:, ::-1], k[:, ::-1], v[:, ::-1], log_g[1], s0_b, False)
    return o_f + o_b[:, ::-1], s_f, s_b


def attend_with_sink(q, k, v, mask, sink):
    b, nq = q.shape[:2]
    qg = q.reshape(b, nq, ATT_KV_HEADS, ATT_GROUP, ATT_HEAD_DIM)
    s = jnp.einsum('bqkgd,bckd->bkgqc', qg, k).astype(jnp.float32) * ATT_SCALE
    s = jnp.where(mask, s, NEG_INF)
    sink_col = jnp.broadcast_to(sink.astype(jnp.float32).reshape(1, ATT_KV_HEADS, ATT_GROUP, 1, 1), s.shape[:-1] + (1,))
    p = jax.nn.softmax(jnp.concatenate([s, sink_col], axis=-1), axis=-1)[..., :-1]
    o = jnp.einsum('bkgqc,bckd->bqkgd', p.astype(v.dtype), v)
    return o.reshape(b, nq, ATT_W)


def window_attention(q, k, v, k_ctx, v_ctx, sink):
    b, seq = q.shape[:2]
    nb = seq // ATT_BLOCK
    band = ATT_BLOCK + 2 * WINDOW
    pad = ((0, 0), (WINDOW, WINDOW), (0, 0), (0, 0))
    kp, vp = jnp.pad(k, pad), jnp.pad(v, pad)
    a = jnp.arange(ATT_BLOCK)
    cidx = jnp.arange(band)
    near = jnp.abs(cidx[None, :] - WINDOW - a[:, None]) <= WINDOW
    ctx_cols = jnp.ones((ATT_BLOCK, k_ctx.shape[1]), bool)

    def block(args):
        q_blk, i = args
        start = i * ATT_BLOCK
        k_blk = lax.dynamic_slice_in_dim(kp, start, band, axis=1)
        v_blk = lax.dynamic_slice_in_dim(vp, start, band, axis=1)
        kpos = start - WINDOW + cidx
        valid = near & ((kpos >= 0) & (kpos < seq))[None, :]
        mask = jnp.concatenate([valid, ctx_cols], axis=1)
        keys = jnp.concatenate([k_blk, k_ctx.astype(k_blk.dtype)], axis=1)
        vals = jnp.concatenate([v_blk, v_ctx.astype(v_blk.dtype)], axis=1)
        return attend_with_sink(q_blk, keys, vals, mask, sink)

    qb = jnp.moveaxis(q.reshape(b, nb, ATT_BLOCK, ATT_HEADS, ATT_HEAD_DIM), 1, 0)
    out = lax.map(block, (qb, jnp.arange(nb)))
    return jnp.moveaxis(out, 0, 1).reshape(b, seq, ATT_W)


def even_mixer(h, w_in, w_out, q_g, k_g, sink, ret_decay, ctx):
    b, seq, _ = h.shape
    proj = h @ w_in
    qr, kr, vr, gr, qa, ka, va = jnp.split(proj, EVEN_SPLITS, axis=-1)
    qr = qr.reshape(b, seq, RET_HEADS, RET_DK)
    kr = kr.reshape(b, seq, RET_HEADS, RET_DK)
    vr = vr.reshape(b, seq, RET_HEADS, RET_DV)
    qa = rms_norm(qa.reshape(b, seq, ATT_HEADS, ATT_HEAD_DIM), q_g)
    ka = rms_norm(ka.reshape(b, seq, ATT_KV_HEADS, ATT_HEAD_DIM), k_g)
    va = va.reshape(b, seq, ATT_KV_HEADS, ATT_HEAD_DIM)
    if ctx is not None:
        qr, kr, qa, ka = rope_2d(qr), rope_2d(kr), rope_2d(qa), rope_2d(ka)
    kr = kr * RET_DK ** -0.5
    log_g = -jnp.exp(ret_decay.astype(jnp.float32))
    if ctx is None:
        zero = jnp.zeros((b, RET_HEADS, RET_DK, RET_DV), jnp.float32)
        s0_f, s0_b = zero, zero
    else:
        s0_f, s0_b = ctx[2], ctx[3]
    o_r, s_f, s_b = bidir_retention(qr, kr, vr, log_g, s0_f, s0_b)
    o_r = head_rms(o_r, h.dtype).reshape(b, seq, RET_W) * jax.nn.silu(gr)
    if ctx is None:
        o_a = attend_with_sink(qa, ka, va, jnp.ones((seq, seq), bool), sink)
        new = (ka, va, s_f.astype(h.dtype), s_b.astype(h.dtype))
    else:
        o_a = window_attention(qa, ka, va, ctx[0], ctx[1], sink)
        new = None
    out = jnp.concatenate([o_r, o_a], axis=-1) @ w_out
    return out, new


def gmlp_mixer(h, w_in, v_g, sw, sb, w_out):
    b, seq, _ = h.shape
    z = jax.nn.gelu(h @ w_in)
    u, v = jnp.split(z, 2, axis=-1)
    v = layer_norm(v, v_g)
    vc = v.reshape(b, seq // GMLP_CHUNK, GMLP_CHUNK, GMLP_GROUPS, GMLP_W // GMLP_GROUPS)
    mixed = jnp.einsum('gpq,bnqgd->bnpgd', sw, vc) + sb.T[None, None, :, :, None]
    return (u * mixed.reshape(b, seq, GMLP_W)) @ w_out


def swiglu(h, w1, w3, w2):
    return (jax.nn.silu(h @ w1) * (h @ w3)) @ w2


def moe_swiglu(h, router, w1, w3, w2):
    probs = jax.nn.softmax((h @ router).astype(jnp.float32), axis=-1)
    top_p, top_i = lax.top_k(probs, TOP_K)
    top_p = top_p / jnp.sum(top_p, axis=-1, keepdims=True)
    gates = jnp.sum(jax.nn.one_hot(top_i, N_EXPERTS, dtype=jnp.float32) * top_p[..., None], axis=-2)
    gates = gates.astype(h.dtype)
    y = jnp.zeros_like(h)
    for e in range(N_EXPERTS):
        y = y + gates[..., e:e + 1] * swiglu(h, w1[e], w3[e], w2[e])
    return y


def setup_inputs(seed: int = 0) -> dict:
    key = jax.random.key(seed)
    ks = jax.random.split(key, 32)
    D = D_MODEL

    def nrm(k, shape, scale):
        return jax.random.normal(k, shape, jnp.float32) * scale

    decay_base = jnp.log(-jnp.log1p(-jnp.power(2.0, -5.0 - jnp.arange(RET_HEADS, dtype=jnp.float32))))
    return {
        'x_prompt': nrm(ks[0], (BATCH, SEQ, D), 1.0),
        'x_sample': nrm(ks[1], (DEC_BATCH, DEC_SEQ, D), 1.0),
        'cache_attn_k': nrm(ks[2], (DEC_BATCH, N_EVEN, PAST_LEN, ATT_KV_HEADS, ATT_HEAD_DIM), 1.0),
        'cache_attn_v': nrm(ks[3], (DEC_BATCH, N_EVEN, PAST_LEN, ATT_KV_HEADS, ATT_HEAD_DIM), 1.0),
        'state_ret_fwd': nrm(ks[4], (DEC_BATCH, N_EVEN, RET_HEADS, RET_DK, RET_DV), 1.0),
        'state_ret_bwd': nrm(ks[5], (DEC_BATCH, N_EVEN, RET_HEADS, RET_DK, RET_DV), 1.0),
        'c': nrm(ks[6], (DEC_BATCH, D), 1.0),
        'c_ctx': nrm(ks[7], (D,), 1.0),
        'norm_g': 1.0 + nrm(ks[8], (DEPTH, 2, D), 0.02),
        'ada_w': nrm(ks[9], (DEPTH, D, N_MOD * D), ADA_SCALE * D ** -0.5),
        'ada_b': nrm(ks[10], (DEPTH, N_MOD * D), 0.02),
        'ev_w_in': nrm(ks[11], (N_EVEN, D, IN_EVEN), D ** -0.5),
        'ev_w_out': nrm(ks[12], (N_EVEN, MIX_W, D), MIX_W ** -0.5),
        'ev_q_norm': 1.0 + nrm(ks[13], (N_EVEN, ATT_HEAD_DIM), 0.02),
        'ev_k_norm': 1.0 + nrm(ks[14], (N_EVEN, ATT_HEAD_DIM), 0.02),
        'ev_sink': nrm(ks[15], (N_EVEN, ATT_HEADS), 0.5),
        'ev_ret_decay': decay_base + nrm(ks[16], (N_EVEN, 2, RET_HEADS), 0.05),
        'ffn_w1': nrm(ks[17], (N_EVEN, D, FFN_DIM), D ** -0.5),
        'ffn_w3': nrm(ks[18], (N_EVEN, D, FFN_DIM), D ** -0.5),
        'ffn_w2': nrm(ks[19], (N_EVEN, FFN_DIM, D), FFN_DIM ** -0.5),
        'od_w_in': nrm(ks[20], (N_ODD, D, 2 * GMLP_W), D ** -0.5),
        'od_v_norm': 1.0 + nrm(ks[21], (N_ODD, GMLP_W), 0.02),
        'od_spatial_w': nrm(ks[22], (N_ODD, GMLP_GROUPS, GMLP_CHUNK, GMLP_CHUNK), 0.5 * GMLP_CHUNK ** -0.5),
        'od_spatial_b': 1.0 + nrm(ks[23], (N_ODD, GMLP_GROUPS, GMLP_CHUNK), 0.02),
        'od_w_out': nrm(ks[24], (N_ODD, GMLP_W, D), GMLP_W ** -0.5),
        'moe_router': nrm(ks[25], (N_ODD, D, N_EXPERTS), D ** -0.5),
        'moe_w1': nrm(ks[26], (N_ODD, N_EXPERTS, D, EXPERT_DIM), D ** -0.5),
        'moe_w3': nrm(ks[27], (N_ODD, N_EXPERTS, D, EXPERT_DIM), D ** -0.5),
        'moe_w2': nrm(ks[28], (N_ODD, N_EXPERTS, EXPERT_DIM, D), EXPERT_DIM ** -0.5),
    }


def reference(x_prompt, x_sample, cache_attn_k, cache_attn_v, state_ret_fwd, state_ret_bwd, c, c_ctx,
              norm_g, ada_w, ada_b, ev_w_in, ev_w_out, ev_q_norm, ev_k_norm, ev_sink, ev_ret_decay,
              ffn_w1, ffn_w3, ffn_w2, od_w_in, od_v_norm, od_spatial_w, od_spatial_b, od_w_out,
              moe_router, moe_w1, moe_w3, moe_w2):

    def run_trunk(x, cond, caches):
        ks, vs, sfs, sbs = [], [], [], []
        for l in range(DEPTH):
            sh1, sc1, g1, sh2, sc2, g2 = adaln(cond, ada_w[l], ada_b[l])
            j = l // 2
            h = modulate(x, norm_g[l, 0], sh1, sc1)
            if l % 2 == 0:
                ctx = None if caches is None else (caches[0][:, j], caches[1][:, j], caches[2][:, j], caches[3][:, j])
                mix, new = even_mixer(h, ev_w_in[j], ev_w_out[j], ev_q_norm[j], ev_k_norm[j],
                                      ev_sink[j], ev_ret_decay[j], ctx)
                if new is not None:
                    ks.append(new[0])
                    vs.append(new[1])
                    sfs.append(new[2])
                    sbs.append(new[3])
            else:
                mix = gmlp_mixer(h, od_w_in[j], od_v_norm[j], od_spatial_w[j], od_spatial_b[j], od_w_out[j])
            x = x + g1 * mix
            h = modulate(x, norm_g[l, 1], sh2, sc2)
            if l % 2 == 0:
                ff = swiglu(h, ffn_w1[j], ffn_w3[j], ffn_w2[j])
            else:
                ff = moe_swiglu(h, moe_router[j], moe_w1[j], moe_w3[j], moe_w2[j])
            x = x + g2 * ff
        return x, ks, vs, sfs, sbs

    y_prompt, ks, vs, sfs, sbs = run_trunk(x_prompt, c_ctx[None, :], None)
    new_cache_attn_k = jnp.stack(ks, axis=1)
    new_cache_attn_v = jnp.stack(vs, axis=1)
    new_state_ret_fwd = jnp.stack(sfs, axis=1)
    new_state_ret_bwd = jnp.stack(sbs, axis=1)
    y_sample, _, _, _, _ = run_trunk(x_sample, c, (cache_attn_k, cache_attn_v, state_ret_fwd, state_ret_bwd))
    return (y_prompt, y_sample, new_cache_attn_k, new_cache_attn_v, new_state_ret_fwd, new_state_ret_bwd)
```

```python
import numpy as np
import ml_dtypes
from contextlib import ExitStack
import concourse.bass as bass
import concourse.mybir as mybir
from concourse.bass_utils import run_bass_kernel_spmd

F32 = mybir.dt.float32
BF16 = mybir.dt.bfloat16
AF = mybir.ActivationFunctionType
ALU = mybir.AluOpType
EPS = 1e-6


class Cfg:
    def __init__(s, D=4096, SEQ=256, DSEQ=4096, PAST=512, FFN=11008, EXPD=2048, NE=8, GRID_W=64):
        s.D, s.SEQ, s.DSEQ, s.PAST, s.FFN, s.EXPD, s.NE, s.GRID_W = D, SEQ, DSEQ, PAST, FFN, EXPD, NE, GRID_W
        s.KC = D // 128
        s.PB = 2
        assert s.PB * SEQ == 512
        s.T = 512 + DSEQ
        s.NT = s.T // 512
        s.RH, s.DK, s.DV = 8, 256, 256
        s.AH, s.KVH, s.HD = 16, 4, 128
        s.IN_EVEN = 2 * 2048 + 2 * 2048 + 2048 + 2 * 512
        assert FFN % 128 == 0 and EXPD % 128 == 0


def host_consts(cfg):
    c = {}
    i = np.arange(128)
    ident = np.eye(128, dtype=np.float32)
    MfT = np.maximum(i[None, :] - i[:, None], 0).astype(np.float32)
    MbT = np.maximum(i[:, None] - i[None, :], 0).astype(np.float32)
    IP1 = np.tile((i + 1)[None, :], (128, 1)).astype(np.float32)
    I128M = np.tile((128 - i)[None, :], (128, 1)).astype(np.float32)
    maskP = (i[:, None] >= i[None, :]).astype(np.float32)
    maskN = (i[:, None] <= i[None, :]).astype(np.float32)
    perm_ret = np.zeros((128, 128), np.float32)
    perm_att = np.zeros((128, 128), np.float32)
    for m in range(128):
        perm_ret[(m + 64) % 128, m] = 1.0
        pa = m + 32 if (m % 64) < 32 else m - 32
        perm_att[pa, m] = 1.0
    cols = np.zeros((128, 128), np.float32)
    cols[:, 0] = 127 - i
    cols[:, 1] = i
    cols[:, 2] = EPS
    c["cst"] = np.concatenate([ident, MfT, MbT, IP1, I128M, np.tile(maskP, (1, 4)), np.tile(maskN, (1, 4)),
                               perm_ret, perm_att, cols], axis=1).astype(np.float32)
    t = np.arange(cfg.DSEQ)
    row = (t // cfg.GRID_W).astype(np.float64)
    col = (t % cfg.GRID_W).astype(np.float64)
    p = np.arange(128)
    inv64 = 10000.0 ** (-(np.arange(64, dtype=np.float64)) / 64)
    f = p % 64
    sign = np.where(p < 64, -1.0, 1.0)
    ang_r = inv64[f][:, None] * row[None, :]
    ang_c = inv64[f][:, None] * col[None, :]
    inv32 = 10000.0 ** (-(np.arange(32, dtype=np.float64)) / 32)
    fa = p % 32
    sign_a = np.where((p % 64) < 32, -1.0, 1.0)
    pos_a = np.where(p[:, None] < 64, row[None, :], col[None, :])
    ang_a = inv32[fa][:, None] * pos_a
    rope = np.stack([np.cos(ang_r), sign[:, None] * np.sin(ang_r), np.cos(ang_c), sign[:, None] * np.sin(ang_c),
                     np.cos(ang_a), sign_a[:, None] * np.sin(ang_a)], axis=0).astype(np.float32)
    c["rope"] = np.ascontiguousarray(rope)
    selc = np.zeros((8, cfg.NE, 128), np.float32)
    for e in range(cfg.NE):
        selc[e, e, :] = 1.0
    c["selc"] = selc.reshape(8, cfg.NE * 128)
    return c


class Tok:
    __slots__ = ("w", "r", "x")

    def __init__(s):
        s.w = None
        s.r = {}
        s.x = False


class Tl:
    __slots__ = ("t", "tok")

    def __init__(s, t):
        s.t = t
        s.tok = Tok()


class Ring:
    def __init__(s, items):
        s.items = items
        s.i = 0

    def next(s):
        it = s.items[s.i % len(s.items)]
        s.i += 1
        return it


class Eng:
    def __init__(s, h, sem, inorder=False):
        s.h, s.sem, s.cnt, s.waited, s.inorder = h, sem, 0, {}, inorder


class Sched:
    def __init__(s, nc, es, n_sp=8, n_gp=6):
        s.nc = nc
        mk = lambda n: es.enter_context(nc.semaphore(n))
        s.PE = Eng(nc.tensor, mk("s_pe"), inorder=True)
        s.ACT = Eng(nc.scalar, mk("s_act"))
        s.DVE = Eng(nc.vector, mk("s_dve"))
        s.SP = Eng(nc.sync, mk("s_sp"))
        s.GP = Eng(nc.gpsimd, mk("s_gp"))
        s.engs = [s.PE, s.ACT, s.DVE, s.SP, s.GP]
        s.SP.ring = [[mk(f"s_spd{i}"), 0] for i in range(n_sp)]
        s.GP.ring = [[mk(f"s_gpd{i}"), 0] for i in range(n_gp)]
        s.SP.ri = 0
        s.GP.ri = 0

    def _wait(s, eng, deps):
        for sem, val in deps:
            if eng.inorder and sem is eng.sem:
                continue
            k = id(sem)
            if eng.waited.get(k, 0) >= val:
                continue
            eng.h.wait_ge(sem, val)
            eng.waited[k] = val

    @staticmethod
    def _deps(reads, writes):
        deps = []
        for t in reads:
            if t.w is not None:
                deps.append(t.w)
            if t.x:
                deps.extend(t.r.values())
        for t in writes:
            if t.w is not None:
                deps.append(t.w)
            deps.extend(t.r.values())
        return deps

    @staticmethod
    def _mark(mark, reads, writes):
        k = id(mark[0])
        for t in reads:
            t.r[k] = mark
        for t in writes:
            t.w = mark
            t.r = {}

    def op(s, eng, fn, reads=(), writes=()):
        s._wait(eng, s._deps(reads, writes))
        inst = fn()
        eng.cnt += 1
        inst.then_inc(eng.sem, 1)
        s._mark((eng.sem, eng.cnt), reads, writes)

    def dma(s, q, out, in_, reads=(), writes=()):
        slot = q.ring[q.ri % len(q.ring)]
        q.ri += 1
        deps = s._deps(reads, writes)
        if slot[1] > 0:
            deps.append((slot[0], 16 * slot[1]))
        s._wait(q, deps)
        inst = q.h.dma_start(out=out, in_=in_)
        slot[1] += 1
        inst.then_inc(slot[0], 16)
        s._mark((slot[0], 16 * slot[1]), reads, writes)

    def barrier(s):
        marks = [(e.sem, e.cnt) for e in s.engs if e.cnt > 0]
        for q in (s.SP, s.GP):
            marks += [(sl[0], 16 * sl[1]) for sl in q.ring if sl[1] > 0]
        for e in s.engs:
            inord = e.inorder
            e.inorder = False
            s._wait(e, marks)
            e.inorder = inord


class _Stop(Exception):
    pass


def build(cfg, stop=None, dbg=None):
    nc = bass.Bass("TRN2", target_bir_lowering=False)
    D, KC, T, NT, DSEQ = cfg.D, cfg.KC, cfg.T, cfg.NT, cfg.DSEQ
    FFN, EXPD, NE, PAST = cfg.FFN, cfg.EXPD, cfg.NE, cfg.PAST

    def din(name, shape, dt=F32):
        return nc.dram_tensor(name, list(shape), dt, kind="ExternalInput").ap()

    def dout(name, shape, dt=F32):
        return nc.dram_tensor(name, list(shape), dt, kind="ExternalOutput").ap()

    def dscr(name, shape, dt):
        return nc.dram_tensor(name, list(shape), dt, kind="Internal").ap()

    xp = din("xp", [512, D])
    xs = din("xs", [DSEQ, D])
    ck = din("ck", [PAST, 512])
    cv = din("cv", [PAST, 512])
    sf_in = din("sf", [8, 256, 256])
    sb_in = din("sb", [8, 256, 256])
    cvec = din("cvec", [2, D])
    norm_g = din("norm_g", [4, D])
    ada_w = din("ada_w", [2, D, 6 * D])
    ada_b = din("ada_b", [2, 6 * D])
    w_in = din("ev_w_in", [D, cfg.IN_EVEN])
    w_out = din("ev_w_out", [4096, D])
    qkn = din("qkn", [2, 128])
    sink = din("ev_sink", [1, 16])
    rdecay = din("ev_ret_decay", [1, 16])
    ffn_w1 = din("ffn_w1", [D, FFN])
    ffn_w3 = din("ffn_w3", [D, FFN])
    ffn_w2 = din("ffn_w2", [FFN, D])
    od_w_in = din("od_w_in", [D, 2 * D])
    od_vn = din("od_v_norm", [1, D])
    od_sw = din("od_spatial_w", [16, 128, 128])
    od_sb = din("od_spatial_b", [1, 16 * 128])
    od_w_out = din("od_w_out", [D, D])
    router = din("moe_router", [D, NE])
    moe_w1 = din("moe_w1", [NE, D, EXPD])
    moe_w3 = din("moe_w3", [NE, D, EXPD])
    moe_w2 = din("moe_w2", [NE, EXPD, D])
    cst = din("cst", [128, 2048])
    selc = din("selc", [8, NE * 128])
    rope = din("rope", [6, 128, DSEQ])

    yp = dout("yp", [512, D])
    ys = dout("ys", [DSEQ, D])
    nk = dout("nk", [512, 512])
    nv = dout("nv", [512, 512])
    nsf = dout("nsf", [2, 8, 256, 256])
    nsb = dout("nsb", [2, 8, 256, 256])

    xT = dscr("xT", [KC, 128, T], F32)
    qr_s = dscr("qr_s", [16, 128, T], BF16)
    kr_s = dscr("kr_s", [16, 128, T], BF16)
    gr_s = dscr("gr_s", [16, 128, T], BF16)
    vr_s = dscr("vr_s", [T, 2048], BF16)
    qa_s = dscr("qa_s", [16, 128, T], BF16)
    ka_s = dscr("ka_s", [4, 128, T], BF16)
    va_s = dscr("va_s", [T, 512], BF16)
    mix_s = dscr("mix_s", [32, 128, T], BF16)
    snap_s = dscr("snap_s", [T // 128, 128, 8 * 2 * 256], BF16)

    NACT = 8

    class WB:
        def __init__(s, name, src, knsl):
            s.src = src
            K, N = src.shape
            s.kct, s.nb, s.knsl = K // 128, N // 128, knsl
            s.nsl = (s.kct + knsl - 1) // knsl
            s.sc = dscr("wb_" + name, [s.nsl, s.nb, 128, knsl * 128], BF16)

        def blk(s, k0, kn, n0):
            assert k0 % s.knsl == 0 and kn <= s.knsl
            return s.sc[k0 // s.knsl, n0 // 128][:, 0:kn * 128].rearrange("p (k c) -> p k c", c=128)

    WB_in = WB("w_in", w_in, KC)
    WB_out = WB("w_out", w_out, 32)
    WB_w1 = WB("ffn_w1", ffn_w1, KC)
    WB_w3 = WB("ffn_w3", ffn_w3, KC)
    WB_w2 = WB("ffn_w2", ffn_w2, NACT)
    WB_oin = WB("od_w_in", od_w_in, KC)
    WB_oout = WB("od_w_out", od_w_out, KC)
    WB_m1 = [WB(f"moe_w1_{e}", moe_w1[e], KC) for e in range(NE)]
    WB_m3 = [WB(f"moe_w3_{e}", moe_w3[e], KC) for e in range(NE)]
    WB_m2 = [WB(f"moe_w2_{e}", moe_w2[e], NACT) for e in range(NE)]
    all_wb = [WB_in, WB_out, WB_w1, WB_w3, WB_w2, WB_oin, WB_oout] + WB_m1 + WB_m3 + WB_m2

    es = ExitStack()

    def _body():
        S = Sched(nc, es)
        PE, ACT, DVE, SP, GP = S.PE, S.ACT, S.DVE, S.SP, S.GP
        op, dma = S.op, S.dma

        def sb(es_, name, shape, dt):
            return Tl(es_.enter_context(nc.sbuf_tensor(name, list(shape), dt)))

        pbanks = [Tl(es.enter_context(nc.psum_tensor(f"pb{i}", [128, 512], F32))) for i in range(8)]
        for pbk in pbanks:
            pbk.tok.x = True
        pm = Ring(pbanks[0:4])
        pa = Ring(pbanks[4:6])
        pt = Ring(pbanks[6:8])

        pc = sb(es, "pc", [128, 512], F32)
        dma(SP, pc.t[:, 0:128], cst[:, 0:128], writes=[pc.tok])
        dma(SP, pc.t[:, 128:512], cst[:, 1664:2048], writes=[pc.tok])
        cs = pc
        ident = pc.t[:, 0:128]
        permr32 = pc.t[:, 128:256]
        perma32 = pc.t[:, 256:384]
        c127mj = pc.t[:, 384:385]
        cj = pc.t[:, 385:386]
        epsc = pc.t[:, 386:387]

        cb = sb(es, "cstb", [128, 128 + 128 + 1024], BF16)
        identb = cb.t[:, 0:128]
        onesb = cb.t[:, 128:256]
        maskPb = cb.t[:, 256:768]
        maskNb = cb.t[:, 768:1280]
        op(DVE, lambda: nc.vector.tensor_copy(out=identb, in_=ident), reads=[pc.tok], writes=[cb.tok])
        op(DVE, lambda: nc.vector.memset(onesb, 1.0), writes=[cb.tok])
        dma(GP, cb.t[:, 256:1280], cst[:, 640:1664], writes=[cb.tok])
        ones32 = sb(es, "ones32", [128, 128], F32)
        op(DVE, lambda: nc.vector.memset(ones32.t[:], 1.0), writes=[ones32.tok])

        prm = sb(es, "prm", [128, 512], F32)
        ng = prm.t[:, 0:4 * KC]
        vg = prm.t[:, 128:128 + KC]
        qg = prm.t[:, 192:193]
        kg = prm.t[:, 193:194]
        modv = [sb(es, f"modv{l}", [128, 6 * KC, 2], F32) for l in range(2)]
        Amod = [[sb(es, f"A{l}{i}", [128, KC, 2], F32) for i in range(2)] for l in range(2)]
        retc = sb(es, "retc", [128, 64], F32)
        lg = retc.t[:, 0:16]
        KD = retc.t[:, 16:32]
        CD = retc.t[:, 32:48]
        esink = retc.t[:, 48:64]

        sbB = sb(es, "sbB", [128, 2048], BF16)
        swT = sb(es, "swT", [128, 16, 128], BF16)
        rt = sb(es, "rt", [128, KC, NE], BF16)

        def load_rows_T(src_rows_ap, nrows, dst_ap, dst_tok, stage):
            dma(SP, stage.t[0:nrows, :], src_rows_ap, writes=[stage.tok])
            ps = pt.next()
            op(PE, lambda: nc.tensor.transpose(ps.t[:, 0:nrows], stage.t[0:nrows, :], ident[0:nrows, 0:nrows]),
               reads=[stage.tok, cs.tok], writes=[ps.tok])
            op(DVE, lambda: nc.vector.tensor_copy(out=dst_ap, in_=ps.t[:, 0:nrows]), reads=[ps.tok], writes=[dst_tok])

        def bcast_row(src_row_ap, n, dst_ap, dst_tok, stage):
            dma(SP, stage.t[0:1, 0:n], src_row_ap, writes=[stage.tok])
            for j0 in range(0, n, 512):
                w = min(512, n - j0)
                ps = pa.next()
                op(PE, lambda: nc.tensor.matmul(ps.t[:, 0:w], ones32.t[0:1, :], stage.t[0:1, j0:j0 + w], start=True, stop=True),
                   reads=[stage.tok, ones32.tok], writes=[ps.tok])
                op(DVE, lambda: nc.vector.tensor_copy(out=dst_ap[:, j0:j0 + w], in_=ps.t[:, 0:w]), reads=[ps.tok], writes=[dst_tok])

        with ExitStack() as ph:
            stg = [sb(ph, f"stg{i}", [128, 128], F32) for i in range(3)]
            stgr = Ring(stg)
            rowst = sb(ph, "rowst", [1, 2048], F32)
            scT = sb(ph, "scT", [128, KC, 2], BF16)
            ctmp = sb(ph, "ctmp", [128, 2 * KC], F32)
            adab = [sb(ph, f"adab{l}", [128, 6 * KC], F32) for l in range(2)]

            ngv = norm_g.rearrange("a (c p) -> (a c) p", p=128)
            for r0 in range(0, 4 * KC, 128):
                nr = min(128, 4 * KC - r0)
                load_rows_T(ngv[r0:r0 + nr, :], nr, ng[:, r0:r0 + nr], prm.tok, stgr.next())
            load_rows_T(od_vn.rearrange("a (c p) -> (a c) p", p=128), KC, vg, prm.tok, stgr.next())
            load_rows_T(qkn, 2, prm.t[:, 192:194], prm.tok, stgr.next())
            load_rows_T(cvec.rearrange("a (c p) -> (a c) p", p=128), 2 * KC, ctmp.t[:, 0:2 * KC], ctmp.tok, stgr.next())
            for cnd in range(2):
                op(ACT, lambda: nc.scalar.activation(out=scT.t[:, :, cnd], in_=ctmp.t[:, cnd * KC:(cnd + 1) * KC], func=AF.Silu),
                   reads=[ctmp.tok], writes=[scT.tok])
            for l in range(2):
                abv = ada_b[l:l + 1, :].rearrange("a (c p) -> (a c) p", p=128)
                for r0 in range(0, 6 * KC, 128):
                    nr = min(128, 6 * KC - r0)
                    load_rows_T(abv[r0:r0 + nr, :], nr, adab[l].t[:, r0:r0 + nr], adab[l].tok, stgr.next())
            bcast_row(rdecay, 16, lg, retc.tok, rowst)
            op(ACT, lambda: nc.scalar.activation(out=lg, in_=lg, func=AF.Exp), reads=[retc.tok], writes=[retc.tok])
            op(DVE, lambda: nc.vector.tensor_scalar(out=lg, in0=lg, scalar1=-1.0, scalar2=None, op0=ALU.mult), reads=[retc.tok], writes=[retc.tok])
            for h in range(8):
                op(ACT, lambda: nc.scalar.activation(out=KD[:, h:h + 1], in_=c127mj, func=AF.Exp, scale=lg[:, h:h + 1]),
                   reads=[cs.tok, retc.tok], writes=[retc.tok])
                op(ACT, lambda: nc.scalar.activation(out=KD[:, 8 + h:9 + h], in_=cj, func=AF.Exp, scale=lg[:, 8 + h:9 + h]),
                   reads=[cs.tok, retc.tok], writes=[retc.tok])
            op(DVE, lambda: nc.vector.tensor_scalar(out=KD, in0=KD, scalar1=0.0625, scalar2=None, op0=ALU.mult), reads=[retc.tok], writes=[retc.tok])
            op(ACT, lambda: nc.scalar.activation(out=CD, in_=lg, func=AF.Exp, scale=128.0), reads=[retc.tok], writes=[retc.tok])
            bcast_row(sink, 16, esink, retc.tok, rowst)
            op(ACT, lambda: nc.scalar.activation(out=esink, in_=esink, func=AF.Exp), reads=[retc.tok], writes=[retc.tok])
            bcast_row(od_sb, 2048, sbB.t[:], sbB.tok, rowst)
            for g in range(16):
                st = stgr.next()
                dma(SP, st.t[:], od_sw[g], writes=[st.tok])
                ps = pt.next()
                op(PE, lambda: nc.tensor.transpose(ps.t[:, 0:128], st.t[:], ident), reads=[st.tok, cs.tok], writes=[ps.tok])
                op(DVE, lambda: nc.vector.tensor_copy(out=swT.t[:, g, :], in_=ps.t[:, 0:128]), reads=[ps.tok], writes=[swT.tok])
            dma(GP, rt.t[:], router.rearrange("(c p) e -> p c e", p=128), writes=[rt.tok])

            wring0 = Ring([sb(ph, f"w0_{i}", [128, KC, 512], BF16) for i in range(3)])
            for l in range(2):
                Wv = ada_w[l].rearrange("(kc p) n -> p kc n", p=128)
                for n4 in range(0, 6 * KC, 4):
                    wb = wring0.next()
                    dma(GP, wb.t[:], Wv[:, :, n4 * 128:(n4 + 4) * 128], writes=[wb.tok])
                    for b in range(4):
                        ni = n4 + b
                        ps = pm.next()
                        for kc in range(KC):
                            op(PE, lambda: nc.tensor.matmul(ps.t[:, 0:2], wb.t[:, kc, b * 128:(b + 1) * 128], scT.t[:, kc, :], start=(kc == 0), stop=(kc == KC - 1)),
                               reads=[wb.tok, scT.tok], writes=[ps.tok])
                        op(DVE, lambda: nc.vector.tensor_scalar(out=modv[l].t[:, ni, :], in0=ps.t[:, 0:2], scalar1=adab[l].t[:, ni:ni + 1], scalar2=None, op0=ALU.add),
                           reads=[ps.tok, adab[l].tok], writes=[modv[l].tok])
                for i in range(2):
                    for cnd in range(2):
                        op(DVE, lambda: nc.vector.scalar_tensor_tensor(
                            out=Amod[l][i].t[:, :, cnd], in0=modv[l].t[:, (3 * i + 1) * KC:(3 * i + 2) * KC, cnd], scalar=1.0,
                            in1=ng[:, (l * 2 + i) * KC:(l * 2 + i + 1) * KC], op0=ALU.add, op1=ALU.mult),
                            reads=[modv[l].tok, prm.tok], writes=[Amod[l][i].tok])
            S.barrier()
            if stop == 0:
                return

        def mod_shift(l, i, c, cnd):
            return modv[l].t[:, (3 * i) * KC + c, cnd:cnd + 1]

        def mod_gate(l, i, c, cnd):
            return modv[l].t[:, (3 * i + 2) * KC + c, cnd:cnd + 1]

        mod_toks = [modv[0].tok, modv[1].tok]

        def modulate(xcur, xtoks, hT, htoks, l, i, cnd, sqr, tmpr, rstd):
            ssp = pa.next()
            for c in range(KC):
                sq = sqr.next()
                op(ACT, lambda: nc.scalar.activation(out=sq.t[:], in_=xcur.t[:, c, :], func=AF.Square), reads=[xtoks[c]], writes=[sq.tok])
                op(PE, lambda: nc.tensor.matmul(ssp.t[:], onesb, sq.t[:], start=(c == 0), stop=(c == KC - 1)), reads=[sq.tok, cb.tok], writes=[ssp.tok])
            op(ACT, lambda: nc.scalar.activation(out=rstd.t[:], in_=ssp.t[:], func=AF.Sqrt, scale=1.0 / D, bias=epsc), reads=[ssp.tok, cs.tok], writes=[rstd.tok])
            op(DVE, lambda: nc.vector.reciprocal(out=rstd.t[:], in_=rstd.t[:]), reads=[rstd.tok], writes=[rstd.tok])
            for c in range(KC):
                tm = tmpr.next()
                op(DVE, lambda: nc.vector.tensor_tensor(out=tm.t[:], in0=xcur.t[:, c, :], in1=rstd.t[:], op=ALU.mult), reads=[xtoks[c], rstd.tok], writes=[tm.tok])
                op(ACT, lambda: nc.scalar.activation(out=hT.t[:, c, :], in_=tm.t[:], func=AF.Identity, scale=Amod[l][i].t[:, c, cnd:cnd + 1], bias=mod_shift(l, i, c, cnd)),
                   reads=[tm.tok, Amod[l][i].tok, mod_toks[l]], writes=[htoks[c]])

        def linear(Wv, k0, kn, col_list, rhs_of, ntok, evac, wring, pring=None):
            pring = pring or pm
            for n0 in col_list:
                wb = wring.next()
                dma(GP, wb.t[:, 0:kn, :], Wv.blk(k0, kn, n0), writes=[wb.tok])
                ps = pring.next()
                for kc in range(kn):
                    rap, rtoks = rhs_of(kc)
                    op(PE, lambda: nc.tensor.matmul(ps.t[:, 0:ntok], wb.t[:, kc, :], rap, start=(kc == 0), stop=(kc == kn - 1)),
                       reads=[wb.tok] + rtoks, writes=[ps.tok])
                evac(n0, ps)

        def toks(n):
            return [Tok() for _ in range(n)]

        with ExitStack() as ph:
            stg = Ring([sb(ph, f"pst{i}", [128, 16, 512], F32) for i in range(2)])
            obr = Ring([sb(ph, f"pob{i}", [128, 4, 32, 128], BF16) for i in range(2)])
            ci = 0
            for wbo in all_wb:
                Wv = wbo.src.rearrange("(kc p) n -> p kc n", p=128)
                N = wbo.nb * 128
                for sl in range(wbo.nsl):
                    k0 = sl * wbo.knsl
                    kn = min(wbo.knsl, wbo.kct - k0)
                    for n0 in range(0, N, 512):
                        w = min(512, N - n0)
                        nb4 = w // 128
                        o = obr.next()
                        for h0 in range(0, kn, 16):
                            hn = min(16, kn - h0)
                            st = stg.next()
                            dma(SP, st.t[:, 0:hn, 0:w], Wv[:, k0 + h0:k0 + h0 + hn, n0:n0 + w], writes=[st.tok])
                            for b in range(nb4):
                                if ci % 2 == 0:
                                    op(DVE, lambda: nc.vector.tensor_copy(out=o.t[:, b, h0:h0 + hn, :], in_=st.t[:, 0:hn, b * 128:(b + 1) * 128]), reads=[st.tok], writes=[o.tok])
                                else:
                                    op(ACT, lambda: nc.scalar.copy(out=o.t[:, b, h0:h0 + hn, :], in_=st.t[:, 0:hn, b * 128:(b + 1) * 128]), reads=[st.tok], writes=[o.tok])
                                ci += 1
                        dst = wbo.sc[sl, n0 // 128:n0 // 128 + nb4][:, :, 0:kn * 128].rearrange("b p f -> p b f")
                        dma(GP, dst, o.t[:, 0:nb4, 0:kn, :].rearrange("p b k c -> p b (k c)"), reads=[o.tok])
            S.barrier()
            if stop == 0.5:
                return

        with ExitStack() as ph:
            xcur = sb(ph, "xcur", [128, KC, 512], F32)
            xtk = toks(KC)
            hT = sb(ph, "hT", [128, KC, 512], BF16)
            htk = toks(KC)
            XH = D // 2
            xtokr = Ring([sb(ph, f"xtok{i}", [128, XH], F32) for i in range(2)])
            sqr = Ring([sb(ph, f"sq{i}", [128, 512], BF16) for i in range(2)])
            tmpr = Ring([sb(ph, f"tm{i}", [128, 512], F32) for i in range(3)])
            rstd = sb(ph, "rstd", [128, 512], F32)
            wring = Ring([sb(ph, f"w1_{i}", [128, KC, 128], BF16) for i in range(3)])
            ropet = sb(ph, "ropet", [128, 6, 512], F32)
            f32r = Ring([sb(ph, f"f32_{i}", [128, 512], F32) for i in range(3)])
            b16r = Ring([sb(ph, f"b16_{i}", [128, 512], BF16) for i in range(4)])
            vtok = sb(ph, "vtok", [128, 4, 512], BF16)
            vatok = sb(ph, "vatok", [128, 4, 512], BF16)
            va32 = sb(ph, "va32", [128, 4, 512], F32)
            ka32 = va32
            Wv_in = WB_in

            for t in range(NT):
                tok0 = t * 512
                cnd = 0 if t == 0 else 1
                is_s = t > 0
                src = xp if t == 0 else xs[(t - 1) * 512:t * 512, :]
                for sub in range(4):
                    for half in range(2):
                        xt_ = xtokr.next()
                        dma(SP, xt_.t[:], src[sub * 128:(sub + 1) * 128, half * XH:(half + 1) * XH], writes=[xt_.tok])
                        for c0 in range(half * (KC // 2), (half + 1) * (KC // 2), 4):
                            nn = min(4, (half + 1) * (KC // 2) - c0)
                            ps = pt.next()
                            for cc in range(nn):
                                lc = c0 + cc - half * (KC // 2)
                                op(PE, lambda: nc.tensor.transpose(ps.t[:, cc * 128:(cc + 1) * 128], xt_.t[:, lc * 128:(lc + 1) * 128], ident),
                                   reads=[xt_.tok, cs.tok], writes=[ps.tok])
                            dst = xcur.t[:, c0:c0 + nn, sub * 128:(sub + 1) * 128]
                            srcp = ps.t[:, 0:nn * 128].rearrange("p (c t) -> p c t", t=128)
                            if (c0 // 4) % 2 == 0:
                                op(ACT, lambda: nc.scalar.copy(out=dst, in_=srcp), reads=[ps.tok], writes=xtk[c0:c0 + nn])
                            else:
                                op(DVE, lambda: nc.vector.tensor_copy(out=dst, in_=srcp), reads=[ps.tok], writes=xtk[c0:c0 + nn])
                dma(SP, xT.rearrange("c p t -> p c t")[:, :, tok0:tok0 + 512], xcur.t[:], reads=xtk)
                if dbg is not None and dbg[0] == 10:
                    S.barrier()
                    return
                if is_s:
                    p0 = (t - 1) * 512
                    dma(SP, ropet.t[:], rope.rearrange("k p t -> p k t")[:, :, p0:p0 + 512], writes=[ropet.tok])
                modulate(xcur, xtk, hT, htk, 0, 0, cnd, sqr, tmpr, rstd)
                if dbg is not None and dbg[0] == 11:
                    S.barrier()
                    return

                def rhs_h(kc):
                    return hT.t[:, kc, :], [htk[kc]]

                def do_rope(x32, perm32, ci, si, out_ap, out_tok):
                    pr = pa.next()
                    op(PE, lambda: nc.tensor.matmul(pr.t[:], perm32, x32.t[:], start=True, stop=True), reads=[x32.tok, cs.tok], writes=[pr.tok])
                    t1 = f32r.next()
                    op(DVE, lambda: nc.vector.tensor_tensor(out=t1.t[:], in0=pr.t[:], in1=ropet.t[:, si, :], op=ALU.mult), reads=[pr.tok, ropet.tok], writes=[t1.tok])
                    op(DVE, lambda: nc.vector.tensor_tensor(out=x32.t[:], in0=x32.t[:], in1=ropet.t[:, ci, :], op=ALU.mult), reads=[x32.tok, ropet.tok], writes=[x32.tok])
                    op(DVE, lambda: nc.vector.tensor_tensor(out=out_ap, in0=x32.t[:], in1=t1.t[:], op=ALU.add), reads=[x32.tok, t1.tok], writes=[out_tok])

                def to_tokmajor(srcT, dst, col0, dt32):
                    ps = pt.next()
                    if dt32:
                        for sub in range(4):
                            op(PE, lambda: nc.tensor.transpose(ps.t[:, sub * 128:(sub + 1) * 128], srcT.t[:, sub * 128:(sub + 1) * 128], ident),
                               reads=[srcT.tok, cs.tok], writes=[ps.tok])
                        pv = ps.t[:, :].rearrange("p (s f) -> p s f", f=128)
                    else:
                        pb = ps.t[:, :].bitcast(BF16)
                        for sub in range(4):
                            op(PE, lambda: nc.tensor.transpose(pb[:, sub * 128:(sub + 1) * 128], srcT.t[:, sub * 128:(sub + 1) * 128], identb),
                               reads=[srcT.tok, cb.tok], writes=[ps.tok])
                        pv = pb[:, 0:512].rearrange("p (s f) -> p s f", f=128)
                    return ps, pv

                def evac_in(n0, ps):
                    n = n0 // 128
                    if n < 32:
                        dst_s = qr_s if n < 16 else kr_s
                        c = n % 16
                        ob = b16r.next()
                        if is_s:
                            x32 = f32r.next()
                            op(ACT, lambda: nc.scalar.copy(out=x32.t[:], in_=ps.t[:]), reads=[ps.tok], writes=[x32.tok])
                            ci = 0 if c % 2 == 0 else 2
                            do_rope(x32, permr32, ci, ci + 1, ob.t[:], ob.tok)
                        else:
                            op(ACT, lambda: nc.scalar.copy(out=ob.t[:], in_=ps.t[:]), reads=[ps.tok], writes=[ob.tok])
                        dma(SP, dst_s[c, :, tok0:tok0 + 512], ob.t[:], reads=[ob.tok])
                    elif n < 48:
                        c = n - 32
                        ob = b16r.next()
                        op(ACT, lambda: nc.scalar.copy(out=ob.t[:], in_=ps.t[:]), reads=[ps.tok], writes=[ob.tok])
                        p2, pv = to_tokmajor(ob, vtok, c * 128, False)
                        op(DVE, lambda: nc.vector.tensor_copy(out=vtok.t[:, :, (c % 4) * 128:(c % 4 + 1) * 128], in_=pv), reads=[p2.tok], writes=[vtok.tok])
                        if c % 4 == 3:
                            dma(SP, vr_s[tok0:tok0 + 512, (c - 3) * 128:(c + 1) * 128].rearrange("(s p) f -> p s f", p=128), vtok.t[:], reads=[vtok.tok])
                    elif n < 64:
                        c = n - 48
                        ob = b16r.next()
                        op(ACT, lambda: nc.scalar.activation(out=ob.t[:], in_=ps.t[:], func=AF.Silu), reads=[ps.tok], writes=[ob.tok])
                        dma(SP, gr_s[c, :, tok0:tok0 + 512], ob.t[:], reads=[ob.tok])
                    elif n < 84:
                        isq = n < 80
                        hh = n - 64 if isq else n - 80
                        gcol = qg if isq else kg
                        sq = sqr.next()
                        op(ACT, lambda: nc.scalar.activation(out=sq.t[:], in_=ps.t[:], func=AF.Square), reads=[ps.tok], writes=[sq.tok])
                        ssp = pa.next()
                        op(PE, lambda: nc.tensor.matmul(ssp.t[:], onesb, sq.t[:], start=True, stop=True), reads=[sq.tok, cb.tok], writes=[ssp.tok])
                        r32 = f32r.next()
                        op(ACT, lambda: nc.scalar.activation(out=r32.t[:], in_=ssp.t[:], func=AF.Sqrt, scale=1.0 / 128, bias=epsc), reads=[ssp.tok, cs.tok], writes=[r32.tok])
                        op(DVE, lambda: nc.vector.reciprocal(out=r32.t[:], in_=r32.t[:]), reads=[r32.tok], writes=[r32.tok])
                        x32 = f32r.next()
                        op(DVE, lambda: nc.vector.scalar_tensor_tensor(out=x32.t[:], in0=ps.t[:], scalar=gcol, in1=r32.t[:], op0=ALU.mult, op1=ALU.mult),
                           reads=[ps.tok, prm.tok, r32.tok], writes=[x32.tok])
                        ob = b16r.next()
                        if is_s:
                            do_rope(x32, perma32, 4, 5, ob.t[:], ob.tok)
                        else:
                            op(ACT, lambda: nc.scalar.copy(out=ob.t[:], in_=x32.t[:]), reads=[x32.tok], writes=[ob.tok])
                            if not isq:
                                p2, pv = to_tokmajor(x32, ka32, hh * 128, True)
                                op(DVE, lambda: nc.vector.tensor_copy(out=ka32.t[:, :, hh * 128:(hh + 1) * 128], in_=pv), reads=[p2.tok], writes=[ka32.tok])
                                if hh == 3:
                                    dma(SP, nk.rearrange("(s p) f -> p s f", p=128), ka32.t[:], reads=[ka32.tok])
                        dst_s = qa_s if isq else ka_s
                        dma(SP, dst_s[hh, :, tok0:tok0 + 512], ob.t[:], reads=[ob.tok])
                    else:
                        hh = n - 84
                        x32 = f32r.next()
                        op(ACT, lambda: nc.scalar.copy(out=x32.t[:], in_=ps.t[:]), reads=[ps.tok], writes=[x32.tok])
                        p2, pv = to_tokmajor(x32, va32, hh * 128, True)
                        op(DVE, lambda: nc.vector.tensor_copy(out=vatok.t[:, :, hh * 128:(hh + 1) * 128], in_=pv), reads=[p2.tok], writes=[vatok.tok])
                        if not is_s:
                            op(ACT, lambda: nc.scalar.copy(out=va32.t[:, :, hh * 128:(hh + 1) * 128], in_=pv), reads=[p2.tok], writes=[va32.tok])
                        if hh == 3:
                            dma(SP, va_s[tok0:tok0 + 512, :].rearrange("(s p) f -> p s f", p=128), vatok.t[:], reads=[vatok.tok])
                            if not is_s:
                                dma(SP, nv.rearrange("(s p) f -> p s f", p=128), va32.t[:], reads=[va32.tok])

                cl = [n * 128 for n in range(88)]
                if dbg is not None and dbg[0] == 12:
                    cl = [n * 128 for n in dbg[1]]
                linear(Wv_in, 0, KC, cl, rhs_h, 512, evac_in, wring)
                if dbg is not None and dbg[0] == 12 and t == dbg[2]:
                    S.barrier()
                    return
            S.barrier()
            if stop == 1:
                return

        seqs = [(0, 2, None), (256, 2, None), (512, DSEQ // 128, 0)]
        with ExitStack() as ph:
            DT = sb(ph, "DT", [128, 8, 128], F32)
            Qf = sb(ph, "Qf", [128, 8, 2, 128], F32)
            Qb = sb(ph, "Qb", [128, 8, 2, 128], F32)
            dec = sb(ph, "dec", [128, 512], F32)
            dma(SP, dec.t[:], cst[:, 128:640], writes=[dec.tok])
            MfT = dec.t[:, 0:128]
            MbT = dec.t[:, 128:256]
            IP1 = dec.t[:, 256:384]
            I128M = dec.t[:, 384:512]
            tmpa = sb(ph, "tmpa", [128, 128], F32)
            tmpb = sb(ph, "tmpb", [128, 128], F32)
            for h in range(8):
                op(DVE, lambda: nc.vector.tensor_scalar(out=tmpa.t[:], in0=MfT, scalar1=lg[:, h:h + 1], scalar2=None, op0=ALU.mult),
                   reads=[dec.tok, retc.tok], writes=[tmpa.tok])
                op(DVE, lambda: nc.vector.scalar_tensor_tensor(out=tmpb.t[:], in0=MbT, scalar=lg[:, 8 + h:9 + h], in1=tmpa.t[:], op0=ALU.mult, op1=ALU.add),
                   reads=[dec.tok, retc.tok, tmpa.tok], writes=[tmpb.tok])
                op(ACT, lambda: nc.scalar.activation(out=tmpa.t[:], in_=tmpb.t[:], func=AF.Exp), reads=[tmpb.tok], writes=[tmpa.tok])
                op(DVE, lambda: nc.vector.tensor_scalar(out=DT.t[:, h, :], in0=tmpa.t[:], scalar1=0.0625, scalar2=None, op0=ALU.mult),
                   reads=[tmpa.tok], writes=[DT.tok])
                for hf in range(2):
                    op(ACT, lambda: nc.scalar.activation(out=Qf.t[:, h, hf, :], in_=IP1, func=AF.Exp, scale=lg[:, h:h + 1]),
                       reads=[dec.tok, retc.tok], writes=[Qf.tok])
                    op(ACT, lambda: nc.scalar.activation(out=Qb.t[:, h, hf, :], in_=I128M, func=AF.Exp, scale=lg[:, 8 + h:9 + h]),
                       reads=[dec.tok, retc.tok], writes=[Qb.tok])
            Sf32 = sb(ph, "Sf32", [128, 8, 2, 256], F32)
            Sfb = sb(ph, "Sfb", [128, 8, 2, 256], BF16)
            sft = toks(8)
            sfbt = toks(8)
            kcr = Ring([sb(ph, f"kc{i}", [128, 16, 128], BF16) for i in range(2)])
            qcr = Ring([sb(ph, f"qc{i}", [128, 16, 128], BF16) for i in range(2)])
            gcr = Ring([sb(ph, f"gc{i}", [128, 16, 128], BF16) for i in range(2)])
            vcr = Ring([sb(ph, f"vc{i}", [128, 2048], BF16) for i in range(2)])
            snr = Ring([sb(ph, f"sn{i}", [128, 8, 2, 256], BF16) for i in range(2)])
            kdr = Ring([sb(ph, f"kd{i}", [128, 256], BF16) for i in range(3)])
            atr = Ring([sb(ph, f"at{i}", [128, 128], BF16) for i in range(3)])
            qfr = Ring([sb(ph, f"qf{i}", [128, 2, 128], BF16) for i in range(3)])
            qbr = Ring([sb(ph, f"qb{i}", [128, 2, 128], BF16) for i in range(3)])
            onr = Ring([sb(ph, f"on{i}", [128, 256], BF16) for i in range(3)])
            jkr = Ring([sb(ph, f"jk{i}", [128, 256], BF16) for i in range(2)])
            smr = Ring([sb(ph, f"sm{i}", [128, 4], F32) for i in range(4)])
            mixr = Ring([sb(ph, f"mx{i}", [128, 16, 128], BF16) for i in range(2)])

            def k_tokmajor(kc_, h, col):
                ps = pt.next()
                pb = ps.t[:, :].bitcast(BF16)
                for hf in range(2):
                    op(PE, lambda: nc.tensor.transpose(pb[:, hf * 128:(hf + 1) * 128], kc_.t[:, 2 * h + hf, :], identb), reads=[kc_.tok, cb.tok], writes=[ps.tok])
                kd = kdr.next()
                op(ACT, lambda: nc.scalar.activation(out=kd.t[:], in_=pb[:, 0:256], func=AF.Copy, scale=KD[:, col:col + 1]), reads=[ps.tok, retc.tok], writes=[kd.tok])
                return kd

            def state_update(kd, vc_, h, col):
                ps = pm.next()
                for hf in range(2):
                    op(PE, lambda: nc.tensor.matmul(ps.t[:, hf * 256:(hf + 1) * 256], kd.t[:, hf * 128:(hf + 1) * 128], vc_.t[:, h * 256:(h + 1) * 256], start=True, stop=True),
                       reads=[kd.tok, vc_.tok], writes=[ps.tok])
                sv = Sf32.t[:, h, :, :].rearrange("p a v -> p (a v)")
                op(DVE, lambda: nc.vector.scalar_tensor_tensor(out=sv, in0=sv, scalar=CD[:, col:col + 1], in1=ps.t[:], op0=ALU.mult, op1=ALU.add),
                   reads=[ps.tok, retc.tok, sft[h]], writes=[sft[h]])
                op(ACT, lambda: nc.scalar.copy(out=Sfb.t[:, h, :, :].rearrange("p a v -> p (a v)"), in_=sv), reads=[sft[h]], writes=[sfbt[h]])

            def init_state(src):
                if src is None:
                    op(DVE, lambda: nc.vector.memset(Sf32.t[:], 0.0), writes=sft)
                    op(DVE, lambda: nc.vector.memset(Sfb.t[:], 0.0), writes=sfbt)
                else:
                    for h in range(8):
                        dma(SP, Sf32.t[:, h, :, :], src[h].rearrange("(a p) v -> p a v", p=128), writes=[sft[h]])
                    op(ACT, lambda: nc.scalar.copy(out=Sfb.t[:], in_=Sf32.t[:]), reads=sft, writes=sfbt)

            for si, (stok0, nch, smp) in enumerate(seqs):
                init_state(None if smp is None else sb_in)
                for n in range(nch - 1, -1, -1):
                    c0 = stok0 + n * 128
                    kc_ = kcr.next()
                    vc_ = vcr.next()
                    dma(SP, kc_.t[:], kr_s.rearrange("c p t -> p c t")[:, :, c0:c0 + 128], writes=[kc_.tok])
                    dma(SP, vc_.t[:], vr_s[c0:c0 + 128, :], writes=[vc_.tok])
                    dma(SP, snap_s[c0 // 128], Sfb.t[:].rearrange("p h a v -> p (h a v)"), reads=sfbt)
                    for h in range(8):
                        kd = k_tokmajor(kc_, h, 8 + h)
                        state_update(kd, vc_, h, 8 + h)
                if smp is None:
                    for h in range(8):
                        dma(SP, nsb[si, h].rearrange("(a p) v -> p a v", p=128), Sf32.t[:, h, :, :], reads=[sft[h]])
                S.barrier()
                if stop == 2:
                    return
                init_state(None if smp is None else sf_in)
                for n in range(nch):
                    c0 = stok0 + n * 128
                    kc_, vc_, qc_, gc_, sn_ = kcr.next(), vcr.next(), qcr.next(), gcr.next(), snr.next()
                    dma(SP, kc_.t[:], kr_s.rearrange("c p t -> p c t")[:, :, c0:c0 + 128], writes=[kc_.tok])
                    dma(SP, qc_.t[:], qr_s.rearrange("c p t -> p c t")[:, :, c0:c0 + 128], writes=[qc_.tok])
                    dma(SP, vc_.t[:], vr_s[c0:c0 + 128, :], writes=[vc_.tok])
                    dma(SP, gc_.t[:], gr_s.rearrange("c p t -> p c t")[:, :, c0:c0 + 128], writes=[gc_.tok])
                    dma(SP, sn_.t[:].rearrange("p h a v -> p (h a v)"), snap_s[c0 // 128], writes=[sn_.tok])
                    mx = mixr.next()
                    for h in range(8):
                        kd = k_tokmajor(kc_, h, h)
                        pat = pa.next()
                        for hf in range(2):
                            op(PE, lambda: nc.tensor.matmul(pat.t[:, 0:128], kc_.t[:, 2 * h + hf, :], qc_.t[:, 2 * h + hf, :], start=(hf == 0), stop=(hf == 1)),
                               reads=[kc_.tok, qc_.tok], writes=[pat.tok])
                        at = atr.next()
                        op(DVE, lambda: nc.vector.tensor_tensor(out=at.t[:], in0=pat.t[:, 0:128], in1=DT.t[:, h, :], op=ALU.mult), reads=[pat.tok, DT.tok], writes=[at.tok])
                        qf_, qb_ = qfr.next(), qbr.next()
                        op(DVE, lambda: nc.vector.tensor_tensor(out=qf_.t[:], in0=qc_.t[:, 2 * h:2 * h + 2, :], in1=Qf.t[:, h, :, :], op=ALU.mult), reads=[qc_.tok, Qf.tok], writes=[qf_.tok])
                        op(DVE, lambda: nc.vector.tensor_tensor(out=qb_.t[:], in0=qc_.t[:, 2 * h:2 * h + 2, :], in1=Qb.t[:, h, :, :], op=ALU.mult), reads=[qc_.tok, Qb.tok], writes=[qb_.tok])
                        po = pm.next()
                        vh = vc_.t[:, h * 256:(h + 1) * 256]
                        op(PE, lambda: nc.tensor.matmul(po.t[:, 0:256], at.t[:], vh, start=True, stop=False), reads=[at.tok, vc_.tok], writes=[po.tok])
                        for hf in range(2):
                            op(PE, lambda: nc.tensor.matmul(po.t[:, 0:256], qf_.t[:, hf, :], Sfb.t[:, h, hf, :], start=False, stop=False), reads=[qf_.tok, sfbt[h]], writes=[po.tok])
                        for hf in range(2):
                            op(PE, lambda: nc.tensor.matmul(po.t[:, 0:256], qb_.t[:, hf, :], sn_.t[:, h, hf, :], start=False, stop=(hf == 1)), reads=[qb_.tok, sn_.tok], writes=[po.tok])
                        state_update(kd, vc_, h, h)
                        jk = jkr.next()
                        sm = smr.next()
                        op(ACT, lambda: nc.scalar.activation(out=jk.t[:], in_=po.t[:, 0:256], func=AF.Square, accum_out=sm.t[:, 0:1]), reads=[po.tok], writes=[jk.tok, sm.tok])
                        op(ACT, lambda: nc.scalar.activation(out=sm.t[:, 1:2], in_=sm.t[:, 0:1], func=AF.Sqrt, scale=1.0 / 256, bias=epsc), reads=[sm.tok, cs.tok], writes=[sm.tok])
                        op(DVE, lambda: nc.vector.reciprocal(out=sm.t[:, 2:3], in_=sm.t[:, 1:2]), reads=[sm.tok], writes=[sm.tok])
                        on = onr.next()
                        op(ACT, lambda: nc.scalar.activation(out=on.t[:], in_=po.t[:, 0:256], func=AF.Copy, scale=sm.t[:, 2:3]), reads=[po.tok, sm.tok], writes=[on.tok])
                        p2 = pt.next()
                        pb = p2.t[:, :].bitcast(BF16)
                        for hf in range(2):
                            op(PE, lambda: nc.tensor.transpose(pb[:, hf * 128:(hf + 1) * 128], on.t[:, hf * 128:(hf + 1) * 128], identb), reads=[on.tok, cb.tok], writes=[p2.tok])
                        op(DVE, lambda: nc.vector.tensor_tensor(out=mx.t[:, 2 * h:2 * h + 2, :], in0=pb[:, 0:256].rearrange("p (a t) -> p a t", t=128), in1=gc_.t[:, 2 * h:2 * h + 2, :], op=ALU.mult),
                           reads=[p2.tok, gc_.tok], writes=[mx.tok])
                    dma(SP, mix_s.rearrange("c p t -> p c t")[:, 0:16, c0:c0 + 128], mx.t[:], reads=[mx.tok])
                if smp is None:
                    for h in range(8):
                        dma(SP, nsf[si, h].rearrange("(a p) v -> p a v", p=128), Sf32.t[:, h, :, :], reads=[sft[h]])
                S.barrier()
                if stop == 3:
                    return

        with ExitStack() as ph:
            kctx = sb(ph, "kctx", [128, 4, PAST], BF16)
            vctx = sb(ph, "vctx", [128, PAST // 128, 4, 130], BF16)
            cstage = sb(ph, "cstage", [128, PAST // 128, 512], BF16)
            op(DVE, lambda: nc.vector.memset(vctx.t[:], 1.0), writes=[vctx.tok])
            dma(GP, cstage.t[:], ck.rearrange("(b p) f -> p b f", p=128), writes=[cstage.tok])
            for b in range(PAST // 128):
                for hk in range(4):
                    p2 = pt.next()
                    pb = p2.t[:, :].bitcast(BF16)
                    op(PE, lambda: nc.tensor.transpose(pb[:, 0:128], cstage.t[:, b, hk * 128:(hk + 1) * 128], identb), reads=[cstage.tok, cb.tok], writes=[p2.tok])
                    op(DVE, lambda: nc.vector.tensor_copy(out=kctx.t[:, hk, b * 128:(b + 1) * 128], in_=pb[:, 0:128]), reads=[p2.tok], writes=[kctx.tok])
            for b in range(PAST // 128):
                dma(GP, vctx.t[:, b, :, 0:128], cv[b * 128:(b + 1) * 128, :].rearrange("p (h d) -> p h d", d=128), writes=[vctx.tok])

            NBUF = 4
            kbr = [sb(ph, f"kb{i}", [128, 4, 128], BF16) for i in range(NBUF)]
            vbr = [sb(ph, f"vb{i}", [128, 4, 130], BF16) for i in range(NBUF)]
            for v_ in vbr:
                op(DVE, lambda: nc.vector.memset(v_.t[:], 1.0), writes=[v_.tok])
            qbr2 = Ring([sb(ph, f"qa{i}", [128, 16, 128], BF16) for i in range(2)])
            ptr = Ring([sb(ph, f"pT{i}", [128, 8, 512], BF16) for i in range(2)])
            oar = Ring([sb(ph, f"oa{i}", [128, 4, 128], BF16) for i in range(2)])
            denr = Ring([sb(ph, f"dn{i}", [128, 8], F32) for i in range(2)])
            mixr = Ring([sb(ph, f"mxa{i}", [128, 16, 128], BF16) for i in range(2)])
            nblk_ctx = PAST // 128

            def load_kv(blk_tok0, slot):
                dma(SP, kbr[slot].t[:], ka_s.rearrange("c p t -> p c t")[:, :, blk_tok0:blk_tok0 + 128], writes=[kbr[slot].tok])
                dma(SP, vbr[slot].t[:, :, 0:128], va_s[blk_tok0:blk_tok0 + 128, :].rearrange("p (h d) -> p h d", d=128), writes=[vbr[slot].tok])

            for si, (stok0, nch, smp) in enumerate(seqs):
                loaded = {}
                for i in range(nch):
                    c0 = stok0 + i * 128
                    if smp is None:
                        blks = [(j, None) for j in range(nch)]
                    else:
                        blks = []
                        if i > 0:
                            blks.append((i - 1, maskPb))
                        blks.append((i, None))
                        if i < nch - 1:
                            blks.append((i + 1, maskNb))
                    for j, _ in blks:
                        if j not in loaded:
                            slot = j % NBUF
                            load_kv(stok0 + j * 128, slot)
                            loaded[j] = slot
                    qa_ = qbr2.next()
                    dma(SP, qa_.t[:], qa_s.rearrange("c p t -> p c t")[:, :, c0:c0 + 128], writes=[qa_.tok])
                    mx = mixr.next()
                    for hk in range(4):
                        keyl = [(kbr[loaded[j]].t[:, hk, :], kbr[loaded[j]].tok, vbr[loaded[j]].t[:, hk, :], vbr[loaded[j]].tok, m) for j, m in blks]
                        if smp is not None:
                            keyl += [(kctx.t[:, hk, b * 128:(b + 1) * 128], kctx.tok, vctx.t[:, b, hk, :], vctx.tok, None) for b in range(nblk_ctx)]
                        pT = ptr.next()
                        qrhs = qa_.t[:, hk * 4:(hk + 1) * 4, :].rearrange("p g t -> p (g t)")
                        for kb, (kap, ktok, vap, vtok_, m) in enumerate(keyl):
                            ps = pm.next()
                            op(PE, lambda: nc.tensor.matmul(ps.t[:], kap, qrhs, start=True, stop=True), reads=[ktok, qa_.tok], writes=[ps.tok])
                            op(ACT, lambda: nc.scalar.activation(out=pT.t[:, kb, :], in_=ps.t[:], func=AF.Exp, scale=float(128 ** -0.5)), reads=[ps.tok], writes=[pT.tok])
                            if m is not None:
                                op(DVE, lambda: nc.vector.tensor_tensor(out=pT.t[:, kb, :], in0=pT.t[:, kb, :], in1=m, op=ALU.mult), reads=[pT.tok, cb.tok], writes=[pT.tok])
                        oa = oar.next()
                        dn = denr.next()
                        for g2 in range(2):
                            po = pm.next()
                            for gg in range(2):
                                g = g2 * 2 + gg
                                for kb, (kap, ktok, vap, vtok_, m) in enumerate(keyl):
                                    op(PE, lambda: nc.tensor.matmul(po.t[:, gg * 130:(gg + 1) * 130], pT.t[:, kb, g * 128:(g + 1) * 128], vap, start=(kb == 0), stop=(kb == len(keyl) - 1)),
                                       reads=[pT.tok, vtok_], writes=[po.tok])
                            pov = po.t[:, 0:260].rearrange("p (g d) -> p g d", d=130)
                            for gg in range(2):
                                g = g2 * 2 + gg
                                hh = hk * 4 + g
                                op(DVE, lambda: nc.vector.tensor_tensor(out=dn.t[:, g:g + 1], in0=pov[:, gg, 128:129], in1=esink[:, hh:hh + 1], op=ALU.add), reads=[po.tok, retc.tok], writes=[dn.tok])
                                op(DVE, lambda: nc.vector.reciprocal(out=dn.t[:, 4 + g:5 + g], in_=dn.t[:, g:g + 1]), reads=[dn.tok], writes=[dn.tok])
                                op(ACT, lambda: nc.scalar.activation(out=oa.t[:, g, :], in_=pov[:, gg, 0:128], func=AF.Copy, scale=dn.t[:, 4 + g:5 + g]), reads=[po.tok, dn.tok], writes=[oa.tok])
                        p2 = pt.next()
                        pb = p2.t[:, :].bitcast(BF16)
                        for g in range(4):
                            op(PE, lambda: nc.tensor.transpose(pb[:, g * 128:(g + 1) * 128], oa.t[:, g, :], identb), reads=[oa.tok, cb.tok], writes=[p2.tok])
                        op(DVE, lambda: nc.vector.tensor_copy(out=mx.t[:, hk * 4:(hk + 1) * 4, :], in_=pb[:, 0:512].rearrange("p (g t) -> p g t", t=128)), reads=[p2.tok], writes=[mx.tok])
                    dma(SP, mix_s.rearrange("c p t -> p c t")[:, 16:32, c0:c0 + 128], mx.t[:], reads=[mx.tok])
            S.barrier()
            if stop == 4:
                return

        with ExitStack() as ph:
            xcur = sb(ph, "xcur3", [128, KC, 512], F32)
            xtk = toks(KC)
            hT = sb(ph, "hT3", [128, KC, 512], BF16)
            htk = toks(KC)
            MC = max(KC, 32)
            vT = sb(ph, "vT3", [128, MC, 512], BF16)
            vtk = toks(MC)
            sqr = Ring([sb(ph, f"sq3_{i}", [128, 512], BF16) for i in range(2)])
            tmpr = Ring([sb(ph, f"tm3_{i}", [128, 512], F32) for i in range(3)])
            rstd = sb(ph, "rstd3", [128, 512], F32)
            mean = sb(ph, "mean3", [128, 512], F32)
            act = sb(ph, "act3", [128, NACT, 512], BF16)
            atk = toks(NACT)
            wringA = Ring([sb(ph, f"wA_{i}", [128, max(KC, 32), 128], BF16) for i in range(3)])
            wringB = Ring([sb(ph, f"wB_{i}", [128, NACT, 128], BF16) for i in range(2)])
            Gt = sb(ph, "G3", [128, 512], F32)
            gl = sb(ph, "gl3", [128, 4, 40], F32)
            gT = sb(ph, "gT3", [8, 512], F32)
            sel = sb(ph, "sel3", [8, NE, 128], F32)
            b16r = Ring([sb(ph, f"b163_{i}", [128, 512], BF16) for i in range(2)])
            dma(SP, sel.t[:].rearrange("k e m -> k (e m)"), selc, writes=[sel.tok])

            Wv_out = WB_out
            Wv_w1 = WB_w1
            Wv_w3 = WB_w3
            Wv_w2 = WB_w2
            Wv_oin = WB_oin
            Wv_oout = WB_oout

            def resid_evac(l, i, cnd):
                def ev(n0, ps):
                    c = n0 // 128
                    op(DVE, lambda: nc.vector.scalar_tensor_tensor(out=xcur.t[:, c, :], in0=ps.t[:], scalar=mod_gate(l, i, c, cnd), in1=xcur.t[:, c, :], op0=ALU.mult, op1=ALU.add),
                       reads=[ps.tok, mod_toks[l], xtk[c]], writes=[xtk[c]])
                return ev

            def glu_slices(W1v, W3v, W2v, hid, l, cnd, gate_fn):
                nh = hid // 128
                for s0 in range(0, nh, NACT):
                    ns = min(NACT, nh - s0)
                    G = gate_fn() if gate_fn else None
                    for j in range(ns):
                        col = (s0 + j) * 128
                        w1b, w3b = wringA.next(), wringA.next()
                        dma(GP, w1b.t[:, 0:KC, :], W1v.blk(0, KC, col), writes=[w1b.tok])
                        dma(GP, w3b.t[:, 0:KC, :], W3v.blk(0, KC, col), writes=[w3b.tok])
                        p1, p3 = pm.next(), pm.next()
                        for kc in range(KC):
                            op(PE, lambda: nc.tensor.matmul(p1.t[:], w1b.t[:, kc, :], hT.t[:, kc, :], start=(kc == 0), stop=(kc == KC - 1)), reads=[w1b.tok, htk[kc]], writes=[p1.tok])
                        for kc in range(KC):
                            op(PE, lambda: nc.tensor.matmul(p3.t[:], w3b.t[:, kc, :], hT.t[:, kc, :], start=(kc == 0), stop=(kc == KC - 1)), reads=[w3b.tok, htk[kc]], writes=[p3.tok])
                        s1 = tmpr.next()
                        op(ACT, lambda: nc.scalar.activation(out=s1.t[:], in_=p1.t[:], func=AF.Silu), reads=[p1.tok], writes=[s1.tok])
                        if G is None:
                            op(DVE, lambda: nc.vector.tensor_tensor(out=act.t[:, j, :], in0=p3.t[:], in1=s1.t[:], op=ALU.mult), reads=[p3.tok, s1.tok], writes=[atk[j]])
                        else:
                            op(DVE, lambda: nc.vector.tensor_tensor(out=s1.t[:], in0=p3.t[:], in1=s1.t[:], op=ALU.mult), reads=[p3.tok, s1.tok], writes=[s1.tok])
                            op(DVE, lambda: nc.vector.tensor_tensor(out=act.t[:, j, :], in0=s1.t[:], in1=G.t[:], op=ALU.mult), reads=[s1.tok, G.tok], writes=[atk[j]])
                    linear(W2v, s0, ns, [n * 128 for n in range(KC)], lambda kc: (act.t[:, kc, :], [atk[kc]]), 512, resid_evac(l, 1, cnd), wringB)

            def gelu_tanh(ps, out_ap, out_tok, extra_mul=None, extra_tok=None):
                a = tmpr.next()
                op(ACT, lambda: nc.scalar.activation(out=a.t[:], in_=ps.t[:], func=AF.Square), reads=[ps.tok], writes=[a.tok])
                op(DVE, lambda: nc.vector.tensor_scalar(out=a.t[:], in0=a.t[:], scalar1=0.044715, scalar2=1.0, op0=ALU.mult, op1=ALU.add), reads=[a.tok], writes=[a.tok])
                op(DVE, lambda: nc.vector.tensor_tensor(out=a.t[:], in0=a.t[:], in1=ps.t[:], op=ALU.mult), reads=[a.tok, ps.tok], writes=[a.tok])
                op(ACT, lambda: nc.scalar.activation(out=a.t[:], in_=a.t[:], func=AF.Sigmoid, scale=1.5957691216057308), reads=[a.tok], writes=[a.tok])
                if extra_mul is None:
                    op(DVE, lambda: nc.vector.tensor_tensor(out=out_ap, in0=a.t[:], in1=ps.t[:], op=ALU.mult), reads=[a.tok, ps.tok], writes=[out_tok])
                else:
                    op(DVE, lambda: nc.vector.tensor_tensor(out=a.t[:], in0=a.t[:], in1=ps.t[:], op=ALU.mult), reads=[a.tok, ps.tok], writes=[a.tok])
                    op(DVE, lambda: nc.vector.tensor_tensor(out=out_ap, in0=a.t[:], in1=extra_mul, op=ALU.mult), reads=[a.tok, extra_tok], writes=[out_tok])

            for t in range(NT):
                tok0 = t * 512
                cnd = 0 if t == 0 else 1
                dma(SP, xcur.t[:], xT.rearrange("c p t -> p c t")[:, :, tok0:tok0 + 512], writes=xtk)
                dma(SP, vT.t[:], mix_s.rearrange("c p t -> p c t")[:, :, tok0:tok0 + 512], writes=vtk)
                linear(Wv_out, 0, 32, [n * 128 for n in range(KC)], lambda kc: (vT.t[:, kc, :], [vtk[kc]]), 512, resid_evac(0, 0, cnd), wringA)
                modulate(xcur, xtk, hT, htk, 0, 1, cnd, sqr, tmpr, rstd)
                glu_slices(Wv_w1, Wv_w3, Wv_w2, FFN, 0, cnd, None)
                modulate(xcur, xtk, hT, htk, 1, 0, cnd, sqr, tmpr, rstd)
                s1p, s2p = pa.next(), pa.next()

                def ev_v(n0, ps):
                    c = (n0 - D) // 128
                    gelu_tanh(ps, vT.t[:, c, :], vtk[c])
                    sq = sqr.next()
                    op(ACT, lambda: nc.scalar.activation(out=sq.t[:], in_=vT.t[:, c, :], func=AF.Square), reads=[vtk[c]], writes=[sq.tok])
                    op(PE, lambda: nc.tensor.matmul(s1p.t[:], onesb, vT.t[:, c, :], start=(c == 0), stop=(c == KC - 1)), reads=[vtk[c], cb.tok], writes=[s1p.tok])
                    op(PE, lambda: nc.tensor.matmul(s2p.t[:], onesb, sq.t[:], start=(c == 0), stop=(c == KC - 1)), reads=[sq.tok, cb.tok], writes=[s2p.tok])

                linear(Wv_oin, 0, KC, [D + n * 128 for n in range(KC)], lambda kc: (hT.t[:, kc, :], [htk[kc]]), 512, ev_v, wringA)
                op(ACT, lambda: nc.scalar.activation(out=mean.t[:], in_=s1p.t[:], func=AF.Copy, scale=1.0 / D), reads=[s1p.tok], writes=[mean.tok])
                m2 = tmpr.next()
                op(DVE, lambda: nc.vector.tensor_tensor(out=m2.t[:], in0=mean.t[:], in1=mean.t[:], op=ALU.mult), reads=[mean.tok], writes=[m2.tok])
                op(DVE, lambda: nc.vector.scalar_tensor_tensor(out=rstd.t[:], in0=s2p.t[:], scalar=1.0 / D, in1=m2.t[:], op0=ALU.mult, op1=ALU.subtract), reads=[s2p.tok, m2.tok], writes=[rstd.tok])
                op(ACT, lambda: nc.scalar.activation(out=rstd.t[:], in_=rstd.t[:], func=AF.Sqrt, bias=epsc), reads=[rstd.tok, cs.tok], writes=[rstd.tok])
                op(DVE, lambda: nc.vector.reciprocal(out=rstd.t[:], in_=rstd.t[:]), reads=[rstd.tok], writes=[rstd.tok])
                for c in range(KC):
                    g = (c * 128) // (D // 16)
                    a = tmpr.next()
                    op(DVE, lambda: nc.vector.tensor_tensor(out=a.t[:], in0=vT.t[:, c, :], in1=mean.t[:], op=ALU.subtract), reads=[vtk[c], mean.tok], writes=[a.tok])
                    vn = b16r.next()
                    op(DVE, lambda: nc.vector.scalar_tensor_tensor(out=vn.t[:], in0=a.t[:], scalar=vg[:, c:c + 1], in1=rstd.t[:], op0=ALU.mult, op1=ALU.mult), reads=[a.tok, prm.tok, rstd.tok], writes=[vn.tok])
                    p2 = pt.next()
                    pb = p2.t[:, :].bitcast(BF16)
                    for sub in range(4):
                        op(PE, lambda: nc.tensor.transpose(pb[:, sub * 128:(sub + 1) * 128], vn.t[:, sub * 128:(sub + 1) * 128], identb), reads=[vn.tok, cb.tok], writes=[p2.tok])
                    vtm = b16r.next()
                    op(ACT, lambda: nc.scalar.copy(out=vtm.t[:], in_=pb[:, 0:512]), reads=[p2.tok], writes=[vtm.tok])
                    pmx = pm.next()
                    for sub in range(4):
                        op(PE, lambda: nc.tensor.matmul(pmx.t[:, sub * 128:(sub + 1) * 128], vtm.t[:, sub * 128:(sub + 1) * 128], swT.t[:, g, :], start=True, stop=True), reads=[vtm.tok, swT.tok], writes=[pmx.tok])
                    for sub in range(4):
                        op(DVE, lambda: nc.vector.tensor_tensor(out=vT.t[:, c, sub * 128:(sub + 1) * 128], in0=pmx.t[:, sub * 128:(sub + 1) * 128], in1=sbB.t[:, g * 128:(g + 1) * 128], op=ALU.add),
                           reads=[pmx.tok, sbB.tok], writes=[vtk[c]])

                def ev_u(n0, ps):
                    c = n0 // 128
                    gelu_tanh(ps, vT.t[:, c, :], vtk[c], extra_mul=vT.t[:, c, :], extra_tok=vtk[c])

                linear(Wv_oin, 0, KC, [n * 128 for n in range(KC)], lambda kc: (hT.t[:, kc, :], [htk[kc]]), 512, ev_u, wringA)
                linear(Wv_oout, 0, KC, [n * 128 for n in range(KC)], lambda kc: (vT.t[:, kc, :], [vtk[kc]]), 512, resid_evac(1, 0, cnd), wringA)
                modulate(xcur, xtk, hT, htk, 1, 1, cnd, sqr, tmpr, rstd)
                pg = pa.next()
                for sub in range(4):
                    pl = pm.next()
                    for kc in range(KC):
                        op(PE, lambda: nc.tensor.matmul(pl.t[:, 0:NE], hT.t[:, kc, sub * 128:(sub + 1) * 128], rt.t[:, kc, :], start=(kc == 0), stop=(kc == KC - 1)), reads=[htk[kc], rt.tok], writes=[pl.tok])
                    L = gl.t[:, sub, 0:8]
                    srt = gl.t[:, sub, 8:16]
                    E = gl.t[:, sub, 16:24]
                    nm = gl.t[:, sub, 24:25]
                    dn = gl.t[:, sub, 25:26]
                    Gs = gl.t[:, sub, 32:40]
                    op(DVE, lambda: nc.vector.tensor_copy(out=L, in_=pl.t[:, 0:NE]), reads=[pl.tok], writes=[gl.tok])
                    op(DVE, lambda: nc.vector.max(out=srt, in_=L), reads=[gl.tok], writes=[gl.tok])
                    op(DVE, lambda: nc.vector.tensor_scalar(out=nm, in0=srt[:, 0:1], scalar1=-1.0, scalar2=None, op0=ALU.mult), reads=[gl.tok], writes=[gl.tok])
                    op(ACT, lambda: nc.scalar.activation(out=E, in_=L, func=AF.Exp, bias=nm), reads=[gl.tok], writes=[gl.tok])
                    op(DVE, lambda: nc.vector.scalar_tensor_tensor(out=E, in0=L, scalar=srt[:, 1:2], in1=E, op0=ALU.is_ge, op1=ALU.mult), reads=[gl.tok], writes=[gl.tok])
                    op(DVE, lambda: nc.vector.tensor_reduce(out=dn, in_=E, axis=mybir.AxisListType.X, op=ALU.add), reads=[gl.tok], writes=[gl.tok])
                    op(DVE, lambda: nc.vector.reciprocal(out=dn, in_=dn), reads=[gl.tok], writes=[gl.tok])
                    op(DVE, lambda: nc.vector.tensor_scalar(out=Gs, in0=E, scalar1=dn, scalar2=None, op0=ALU.mult), reads=[gl.tok], writes=[gl.tok])
                    op(PE, lambda: nc.tensor.transpose(pg.t[0:8, sub * 128:(sub + 1) * 128], Gs, ident), reads=[gl.tok, cs.tok], writes=[pg.tok])
                op(DVE, lambda: nc.vector.tensor_copy(out=gT.t[:], in_=pg.t[0:8, :]), reads=[pg.tok], writes=[gT.tok])
                for e in range(NE):
                    def gate_fn(e=e):
                        pgb = pa.next()
                        op(PE, lambda: nc.tensor.matmul(pgb.t[:], sel.t[:, e, :], gT.t[:], start=True, stop=True), reads=[sel.tok, gT.tok], writes=[pgb.tok])
                        op(ACT, lambda: nc.scalar.copy(out=Gt.t[:], in_=pgb.t[:]), reads=[pgb.tok], writes=[Gt.tok])
                        return Gt
                    glu_slices(WB_m1[e], WB_m3[e], WB_m2[e], EXPD, 1, cnd, gate_fn)
                dst = yp if t == 0 else ys[(t - 1) * 512:t * 512, :]
                for c in range(KC):
                    yt = tmpr.next()
                    ps = pt.next()
                    for sub in range(4):
                        op(PE, lambda: nc.tensor.transpose(ps.t[:, sub * 128:(sub + 1) * 128], xcur.t[:, c, sub * 128:(sub + 1) * 128], ident), reads=[xtk[c], cs.tok], writes=[ps.tok])
                    if c % 2 == 0:
                        op(ACT, lambda: nc.scalar.copy(out=yt.t[:], in_=ps.t[:]), reads=[ps.tok], writes=[yt.tok])
                    else:
                        op(DVE, lambda: nc.vector.tensor_copy(out=yt.t[:], in_=ps.t[:]), reads=[ps.tok], writes=[yt.tok])
                    dma(SP, dst[:, c * 128:(c + 1) * 128].rearrange("(s p) f -> p s f", p=128), yt.t[:, :].rearrange("p (s f) -> p s f", f=128), reads=[yt.tok])
            S.barrier()
            if stop == 5:
                return
    with es:
        try:
            _body()
        except _Stop:
            pass
    return nc


_CFG = Cfg()
_NC_CACHE = {}


def make_in_maps(cfg, inputs, ncores):
    f = lambda a: np.ascontiguousarray(np.asarray(a, dtype=np.float32))
    hc = host_consts(cfg)
    D = cfg.D
    shared = {
        "norm_g": f(inputs["norm_g"]).reshape(4, D),
        "ada_w": f(inputs["ada_w"]),
        "ada_b": f(inputs["ada_b"]),
        "ev_w_in": f(inputs["ev_w_in"])[0],
        "ev_w_out": f(inputs["ev_w_out"])[0],
        "qkn": np.stack([f(inputs["ev_q_norm"])[0], f(inputs["ev_k_norm"])[0]], axis=0),
        "ev_sink": f(inputs["ev_sink"]).reshape(1, 16),
        "ev_ret_decay": f(inputs["ev_ret_decay"]).reshape(1, 16),
        "ffn_w1": f(inputs["ffn_w1"])[0],
        "ffn_w3": f(inputs["ffn_w3"])[0],
        "ffn_w2": f(inputs["ffn_w2"])[0],
        "od_w_in": f(inputs["od_w_in"])[0],
        "od_v_norm": f(inputs["od_v_norm"]).reshape(1, D),
        "od_spatial_w": f(inputs["od_spatial_w"])[0],
        "od_spatial_b": f(inputs["od_spatial_b"]).reshape(1, 16 * 128),
        "od_w_out": f(inputs["od_w_out"])[0],
        "moe_router": f(inputs["moe_router"])[0],
        "moe_w1": f(inputs["moe_w1"])[0],
        "moe_w3": f(inputs["moe_w3"])[0],
        "moe_w2": f(inputs["moe_w2"])[0],
        "cst": hc["cst"],
        "selc": hc["selc"],
        "rope": hc["rope"],
    }
    xp = f(inputs["x_prompt"])
    xs = f(inputs["x_sample"])
    ck = f(inputs["cache_attn_k"])
    cv = f(inputs["cache_attn_v"])
    sf = f(inputs["state_ret_fwd"])
    sbw = f(inputs["state_ret_bwd"])
    c = f(inputs["c"])
    cctx = f(inputs["c_ctx"])
    maps = []
    for i in range(ncores):
        m = dict(shared)
        m["xp"] = xp[2 * i:2 * i + 2].reshape(512, D)
        m["xs"] = xs[i]
        m["ck"] = ck[i, 0].reshape(cfg.PAST, 512)
        m["cv"] = cv[i, 0].reshape(cfg.PAST, 512)
        m["sf"] = sf[i, 0]
        m["sb"] = sbw[i, 0]
        m["cvec"] = np.stack([cctx, c[i]], axis=0)
        maps.append(m)
    return maps


def gather(cfg, results, ncores):
    D = cfg.D
    yp = np.stack([r["yp"].reshape(2, cfg.SEQ, D) for r in results]).reshape(2 * ncores, cfg.SEQ, D)
    ys = np.stack([r["ys"] for r in results])
    nk = np.stack([r["nk"].reshape(2, cfg.SEQ, 4, 128) for r in results]).reshape(2 * ncores, 1, cfg.SEQ, 4, 128)
    nv = np.stack([r["nv"].reshape(2, cfg.SEQ, 4, 128) for r in results]).reshape(2 * ncores, 1, cfg.SEQ, 4, 128)
    nsf = np.stack([r["nsf"] for r in results]).reshape(2 * ncores, 1, 8, 256, 256)
    nsb = np.stack([r["nsb"] for r in results]).reshape(2 * ncores, 1, 8, 256, 256)
    return tuple(np.ascontiguousarray(a.astype(np.float32)) for a in (yp, ys, nk, nv, nsf, nsb))


def kernel(**inputs):
    cfg = _CFG
    if "nc" not in _NC_CACHE:
        _NC_CACHE["nc"] = build(cfg)
    nc = _NC_CACHE["nc"]
    maps = make_in_maps(cfg, inputs, 8)
    res = run_bass_kernel_spmd(nc, maps, core_ids=list(range(8)))
    return gather(cfg, res.results, 8)
```

```python
import numpy as np
import ml_dtypes
from contextlib import ExitStack
import concourse.bass as bass
import concourse.mybir as mybir
from concourse.bass_utils import run_bass_kernel_spmd

F32 = mybir.dt.float32
BF16 = mybir.dt.bfloat16
AF = mybir.ActivationFunctionType
ALU = mybir.AluOpType
EPS = 1e-6


class Cfg:
    def __init__(s, D=4096, SEQ=256, DSEQ=4096, PAST=512, FFN=11008, EXPD=2048, NE=8, GRID_W=64):
        s.D, s.SEQ, s.DSEQ, s.PAST, s.FFN, s.EXPD, s.NE, s.GRID_W = D, SEQ, DSEQ, PAST, FFN, EXPD, NE, GRID_W
        s.KC = D // 128
        s.PB = 2
        assert s.PB * SEQ == 512
        s.T = 512 + DSEQ
        s.NT = s.T // 512
        s.RH, s.DK, s.DV = 8, 256, 256
        s.AH, s.KVH, s.HD = 16, 4, 128
        s.IN_EVEN = 2 * 2048 + 2 * 2048 + 2048 + 2 * 512
        assert FFN % 128 == 0 and EXPD % 128 == 0


def host_consts(cfg):
    c = {}
    i = np.arange(128)
    ident = np.eye(128, dtype=np.float32)
    MfT = np.maximum(i[None, :] - i[:, None], 0).astype(np.float32)
    MbT = np.maximum(i[:, None] - i[None, :], 0).astype(np.float32)
    IP1 = np.tile((i + 1)[None, :], (128, 1)).astype(np.float32)
    I128M = np.tile((128 - i)[None, :], (128, 1)).astype(np.float32)
    maskP = (i[:, None] >= i[None, :]).astype(np.float32)
    maskN = (i[:, None] <= i[None, :]).astype(np.float32)
    perm_ret = np.zeros((128, 128), np.float32)
    perm_att = np.zeros((128, 128), np.float32)
    for m in range(128):
        perm_ret[(m + 64) % 128, m] = 1.0
        pa = m + 32 if (m % 64) < 32 else m - 32
        perm_att[pa, m] = 1.0
    cols = np.zeros((128, 128), np.float32)
    cols[:, 0] = 127 - i
    cols[:, 1] = i
    cols[:, 2] = EPS
    c["cst"] = np.concatenate([ident, MfT, MbT, IP1, I128M, np.tile(maskP, (1, 4)), np.tile(maskN, (1, 4)),
                               perm_ret, perm_att, cols], axis=1).astype(np.float32)
    t = np.arange(cfg.DSEQ)
    row = (t // cfg.GRID_W).astype(np.float64)
    col = (t % cfg.GRID_W).astype(np.float64)
    p = np.arange(128)
    inv64 = 10000.0 ** (-(np.arange(64, dtype=np.float64)) / 64)
    f = p % 64
    sign = np.where(p < 64, -1.0, 1.0)
    ang_r = inv64[f][:, None] * row[None, :]
    ang_c = inv64[f][:, None] * col[None, :]
    inv32 = 10000.0 ** (-(np.arange(32, dtype=np.float64)) / 32)
    fa = p % 32
    sign_a = np.where((p % 64) < 32, -1.0, 1.0)
    pos_a = np.where(p[:, None] < 64, row[None, :], col[None, :])
    ang_a = inv32[fa][:, None] * pos_a
    rope = np.stack([np.cos(ang_r), sign[:, None] * np.sin(ang_r), np.cos(ang_c), sign[:, None] * np.sin(ang_c),
                     np.cos(ang_a), sign_a[:, None] * np.sin(ang_a)], axis=0).astype(np.float32)
    c["rope"] = np.ascontiguousarray(rope)
    selc = np.zeros((8, cfg.NE, 128), np.float32)
    for e in range(cfg.NE):
        selc[e, e, :] = 1.0
    c["selc"] = selc.reshape(8, cfg.NE * 128)
    return c


class Tok:
    __slots__ = ("w", "r", "x")

    def __init__(s):
        s.w = None
        s.r = {}
        s.x = False


class Tl:
    __slots__ = ("t", "tok")

    def __init__(s, t):
        s.t = t
        s.tok = Tok()


class Ring:
    def __init__(s, items):
        s.items = items
        s.i = 0

    def next(s):
        it = s.items[s.i % len(s.items)]
        s.i += 1
        return it


class Eng:
    def __init__(s, h, sem, inorder=False):
        s.h, s.sem, s.cnt, s.waited, s.inorder = h, sem, 0, {}, inorder


class Sched:
    def __init__(s, nc, es, n_sp=8, n_gp=6):
        s.nc = nc
        mk = lambda n: es.enter_context(nc.semaphore(n))
        s.PE = Eng(nc.tensor, mk("s_pe"), inorder=True)
        s.ACT = Eng(nc.scalar, mk("s_act"))
        s.DVE = Eng(nc.vector, mk("s_dve"))
        s.SP = Eng(nc.sync, mk("s_sp"))
        s.GP = Eng(nc.gpsimd, mk("s_gp"))
        s.engs = [s.PE, s.ACT, s.DVE, s.SP, s.GP]
        s.SP.ring = [[mk(f"s_spd{i}"), 0] for i in range(n_sp)]
        s.GP.ring = [[mk(f"s_gpd{i}"), 0] for i in range(n_gp)]
        s.SP.ri = 0
        s.GP.ri = 0

    def _wait(s, eng, deps):
        for sem, val in deps:
            if eng.inorder and sem is eng.sem:
                continue
            k = id(sem)
            if eng.waited.get(k, 0) >= val:
                continue
            eng.h.wait_ge(sem, val)
            eng.waited[k] = val

    @staticmethod
    def _deps(reads, writes):
        deps = []
        for t in reads:
            if t.w is not None:
                deps.append(t.w)
            if t.x:
                deps.extend(t.r.values())
        for t in writes:
            if t.w is not None:
                deps.append(t.w)
            deps.extend(t.r.values())
        return deps

    @staticmethod
    def _mark(mark, reads, writes):
        k = id(mark[0])
        for t in reads:
            t.r[k] = mark
        for t in writes:
            t.w = mark
            t.r = {}

    def op(s, eng, fn, reads=(), writes=()):
        s._wait(eng, s._deps(reads, writes))
        inst = fn()
        eng.cnt += 1
        inst.then_inc(eng.sem, 1)
        s._mark((eng.sem, eng.cnt), reads, writes)

    def dma(s, q, out, in_, reads=(), writes=()):
        slot = q.ring[q.ri % len(q.ring)]
        q.ri += 1
        deps = s._deps(reads, writes)
        if slot[1] > 0:
            deps.append((slot[0], 16 * slot[1]))
        s._wait(q, deps)
        inst = q.h.dma_start(out=out, in_=in_)
        slot[1] += 1
        inst.then_inc(slot[0], 16)
        s._mark((slot[0], 16 * slot[1]), reads, writes)

    def barrier(s):
        marks = [(e.sem, e.cnt) for e in s.engs if e.cnt > 0]
        for q in (s.SP, s.GP):
            marks += [(sl[0], 16 * sl[1]) for sl in q.ring if sl[1] > 0]
        for e in s.engs:
            inord = e.inorder
            e.inorder = False
            s._wait(e, marks)
            e.inorder = inord


class _Stop(Exception):
    pass


def build(cfg, stop=None, dbg=None):
    nc = bass.Bass("TRN2", target_bir_lowering=False)
    D, KC, T, NT, DSEQ = cfg.D, cfg.KC, cfg.T, cfg.NT, cfg.DSEQ
    FFN, EXPD, NE, PAST = cfg.FFN, cfg.EXPD, cfg.NE, cfg.PAST

    def din(name, shape, dt=F32):
        return nc.dram_tensor(name, list(shape), dt, kind="ExternalInput").ap()

    def dout(name, shape, dt=F32):
        return nc.dram_tensor(name, list(shape), dt, kind="ExternalOutput").ap()

    def dscr(name, shape, dt):
        return nc.dram_tensor(name, list(shape), dt, kind="Internal").ap()

    xp = din("xp", [512, D])
    xs = din("xs", [DSEQ, D])
    ck = din("ck", [PAST, 512])
    cv = din("cv", [PAST, 512])
    sf_in = din("sf", [8, 256, 256])
    sb_in = din("sb", [8, 256, 256])
    cvec = din("cvec", [2, D])
    norm_g = din("norm_g", [4, D])
    ada_w = din("ada_w", [2, D, 6 * D])
    ada_b = din("ada_b", [2, 6 * D])
    w_in = din("ev_w_in", [D, cfg.IN_EVEN])
    w_out = din("ev_w_out", [4096, D])
    qkn = din("qkn", [2, 128])
    sink = din("ev_sink", [1, 16])
    rdecay = din("ev_ret_decay", [1, 16])
    ffn_w1 = din("ffn_w1", [D, FFN])
    ffn_w3 = din("ffn_w3", [D, FFN])
    ffn_w2 = din("ffn_w2", [FFN, D])
    od_w_in = din("od_w_in", [D, 2 * D])
    od_vn = din("od_v_norm", [1, D])
    od_sw = din("od_spatial_w", [16, 128, 128])
    od_sb = din("od_spatial_b", [1, 16 * 128])
    od_w_out = din("od_w_out", [D, D])
    router = din("moe_router", [D, NE])
    moe_w1 = din("moe_w1", [NE, D, EXPD])
    moe_w3 = din("moe_w3", [NE, D, EXPD])
    moe_w2 = din("moe_w2", [NE, EXPD, D])
    cst = din("cst", [128, 2048])
    selc = din("selc", [8, NE * 128])
    rope = din("rope", [6, 128, DSEQ])

    yp = dout("yp", [512, D])
    ys = dout("ys", [DSEQ, D])
    nk = dout("nk", [512, 512])
    nv = dout("nv", [512, 512])
    nsf = dout("nsf", [2, 8, 256, 256])
    nsb = dout("nsb", [2, 8, 256, 256])

    xT = dscr("xT", [KC, 128, T], F32)
    qr_s = dscr("qr_s", [16, 128, T], BF16)
    kr_s = dscr("kr_s", [16, 128, T], BF16)
    gr_s = dscr("gr_s", [16, 128, T], BF16)
    vr_s = dscr("vr_s", [T, 2048], BF16)
    qa_s = dscr("qa_s", [16, 128, T], BF16)
    ka_s = dscr("ka_s", [4, 128, T], BF16)
    va_s = dscr("va_s", [T, 512], BF16)
    mix_s = dscr("mix_s", [32, 128, T], BF16)
    snap_s = dscr("snap_s", [T // 128, 128, 8 * 2 * 256], BF16)

    NACT = 8

    class WB:
        def __init__(s, name, src, knsl):
            s.src = src
            K, N = src.shape
            s.kct, s.nb, s.knsl = K // 128, N // 128, knsl
            s.nsl = (s.kct + knsl - 1) // knsl
            s.sc = dscr("wb_" + name, [s.nsl, s.nb, 128, knsl * 128], BF16)

        def blk(s, k0, kn, n0):
            assert k0 % s.knsl == 0 and kn <= s.knsl
            return s.sc[k0 // s.knsl, n0 // 128][:, 0:kn * 128].rearrange("p (k c) -> p k c", c=128)

    WB_in = WB("w_in", w_in, KC)
    WB_out = WB("w_out", w_out, 32)
    WB_w1 = WB("ffn_w1", ffn_w1, KC)
    WB_w3 = WB("ffn_w3", ffn_w3, KC)
    WB_w2 = WB("ffn_w2", ffn_w2, 32)
    WB_oin = WB("od_w_in", od_w_in, KC)
    WB_oout = WB("od_w_out", od_w_out, KC)
    WB_m1 = [WB(f"moe_w1_{e}", moe_w1[e], KC) for e in range(NE)]
    WB_m3 = [WB(f"moe_w3_{e}", moe_w3[e], KC) for e in range(NE)]
    WB_m2 = [WB(f"moe_w2_{e}", moe_w2[e], min(32, EXPD // 128)) for e in range(NE)]
    all_wb = [WB_in, WB_out, WB_w1, WB_w3, WB_w2, WB_oin, WB_oout] + WB_m1 + WB_m3 + WB_m2

    es = ExitStack()

    def _body():
        S = Sched(nc, es)
        PE, ACT, DVE, SP, GP = S.PE, S.ACT, S.DVE, S.SP, S.GP
        op, dma = S.op, S.dma

        def sb(es_, name, shape, dt):
            return Tl(es_.enter_context(nc.sbuf_tensor(name, list(shape), dt)))

        pbanks = [Tl(es.enter_context(nc.psum_tensor(f"pb{i}", [128, 512], F32))) for i in range(8)]
        for pbk in pbanks:
            pbk.tok.x = True
        pm = Ring(pbanks[0:4])
        pa = Ring(pbanks[4:6])
        pt = Ring(pbanks[6:8])

        pc = sb(es, "pc", [128, 512], F32)
        dma(SP, pc.t[:, 0:128], cst[:, 0:128], writes=[pc.tok])
        dma(SP, pc.t[:, 128:512], cst[:, 1664:2048], writes=[pc.tok])
        cs = pc
        ident = pc.t[:, 0:128]
        permr32 = pc.t[:, 128:256]
        perma32 = pc.t[:, 256:384]
        c127mj = pc.t[:, 384:385]
        cj = pc.t[:, 385:386]
        epsc = pc.t[:, 386:387]

        cb = sb(es, "cstb", [128, 128 + 128 + 1024], BF16)
        identb = cb.t[:, 0:128]
        onesb = cb.t[:, 128:256]
        maskPb = cb.t[:, 256:768]
        maskNb = cb.t[:, 768:1280]
        op(DVE, lambda: nc.vector.tensor_copy(out=identb, in_=ident), reads=[pc.tok], writes=[cb.tok])
        op(DVE, lambda: nc.vector.memset(onesb, 1.0), writes=[cb.tok])
        dma(GP, cb.t[:, 256:1280], cst[:, 640:1664], writes=[cb.tok])
        ones32 = sb(es, "ones32", [128, 128], F32)
        op(DVE, lambda: nc.vector.memset(ones32.t[:], 1.0), writes=[ones32.tok])

        prm = sb(es, "prm", [128, 512], F32)
        ng = prm.t[:, 0:4 * KC]
        vg = prm.t[:, 128:128 + KC]
        qg = prm.t[:, 192:193]
        kg = prm.t[:, 193:194]
        modv = [sb(es, f"modv{l}", [128, 6 * KC, 2], F32) for l in range(2)]
        Amod = [[sb(es, f"A{l}{i}", [128, KC, 2], F32) for i in range(2)] for l in range(2)]
        retc = sb(es, "retc", [128, 64], F32)
        lg = retc.t[:, 0:16]
        KD = retc.t[:, 16:32]
        CD = retc.t[:, 32:48]
        esink = retc.t[:, 48:64]

        sbB = sb(es, "sbB", [128, 2048], BF16)
        swT = sb(es, "swT", [128, 16, 128], BF16)
        rt = sb(es, "rt", [128, KC, NE], BF16)

        def load_rows_T(src_rows_ap, nrows, dst_ap, dst_tok, stage):
            dma(SP, stage.t[0:nrows, :], src_rows_ap, writes=[stage.tok])
            ps = pt.next()
            op(PE, lambda: nc.tensor.transpose(ps.t[:, 0:nrows], stage.t[0:nrows, :], ident[0:nrows, 0:nrows]),
               reads=[stage.tok, cs.tok], writes=[ps.tok])
            op(DVE, lambda: nc.vector.tensor_copy(out=dst_ap, in_=ps.t[:, 0:nrows]), reads=[ps.tok], writes=[dst_tok])

        def bcast_row(src_row_ap, n, dst_ap, dst_tok, stage):
            dma(SP, stage.t[0:1, 0:n], src_row_ap, writes=[stage.tok])
            for j0 in range(0, n, 512):
                w = min(512, n - j0)
                ps = pa.next()
                op(PE, lambda: nc.tensor.matmul(ps.t[:, 0:w], ones32.t[0:1, :], stage.t[0:1, j0:j0 + w], start=True, stop=True),
                   reads=[stage.tok, ones32.tok], writes=[ps.tok])
                op(DVE, lambda: nc.vector.tensor_copy(out=dst_ap[:, j0:j0 + w], in_=ps.t[:, 0:w]), reads=[ps.tok], writes=[dst_tok])

        with ExitStack() as ph:
            stg = [sb(ph, f"stg{i}", [128, 128], F32) for i in range(3)]
            stgr = Ring(stg)
            rowst = sb(ph, "rowst", [1, 2048], F32)
            scT = sb(ph, "scT", [128, KC, 2], BF16)
            ctmp = sb(ph, "ctmp", [128, 2 * KC], F32)
            adab = [sb(ph, f"adab{l}", [128, 6 * KC], F32) for l in range(2)]

            ngv = norm_g.rearrange("a (c p) -> (a c) p", p=128)
            for r0 in range(0, 4 * KC, 128):
                nr = min(128, 4 * KC - r0)
                load_rows_T(ngv[r0:r0 + nr, :], nr, ng[:, r0:r0 + nr], prm.tok, stgr.next())
            load_rows_T(od_vn.rearrange("a (c p) -> (a c) p", p=128), KC, vg, prm.tok, stgr.next())
            load_rows_T(qkn, 2, prm.t[:, 192:194], prm.tok, stgr.next())
            load_rows_T(cvec.rearrange("a (c p) -> (a c) p", p=128), 2 * KC, ctmp.t[:, 0:2 * KC], ctmp.tok, stgr.next())
            for cnd in range(2):
                op(ACT, lambda: nc.scalar.activation(out=scT.t[:, :, cnd], in_=ctmp.t[:, cnd * KC:(cnd + 1) * KC], func=AF.Silu),
                   reads=[ctmp.tok], writes=[scT.tok])
            for l in range(2):
                abv = ada_b[l:l + 1, :].rearrange("a (c p) -> (a c) p", p=128)
                for r0 in range(0, 6 * KC, 128):
                    nr = min(128, 6 * KC - r0)
                    load_rows_T(abv[r0:r0 + nr, :], nr, adab[l].t[:, r0:r0 + nr], adab[l].tok, stgr.next())
            bcast_row(rdecay, 16, lg, retc.tok, rowst)
            op(ACT, lambda: nc.scalar.activation(out=lg, in_=lg, func=AF.Exp), reads=[retc.tok], writes=[retc.tok])
            op(DVE, lambda: nc.vector.tensor_scalar(out=lg, in0=lg, scalar1=-1.0, scalar2=None, op0=ALU.mult), reads=[retc.tok], writes=[retc.tok])
            for h in range(8):
                op(ACT, lambda: nc.scalar.activation(out=KD[:, h:h + 1], in_=c127mj, func=AF.Exp, scale=lg[:, h:h + 1]),
                   reads=[cs.tok, retc.tok], writes=[retc.tok])
                op(ACT, lambda: nc.scalar.activation(out=KD[:, 8 + h:9 + h], in_=cj, func=AF.Exp, scale=lg[:, 8 + h:9 + h]),
                   reads=[cs.tok, retc.tok], writes=[retc.tok])
            op(DVE, lambda: nc.vector.tensor_scalar(out=KD, in0=KD, scalar1=0.0625, scalar2=None, op0=ALU.mult), reads=[retc.tok], writes=[retc.tok])
            op(ACT, lambda: nc.scalar.activation(out=CD, in_=lg, func=AF.Exp, scale=128.0), reads=[retc.tok], writes=[retc.tok])
            bcast_row(sink, 16, esink, retc.tok, rowst)
            op(ACT, lambda: nc.scalar.activation(out=esink, in_=esink, func=AF.Exp), reads=[retc.tok], writes=[retc.tok])
            bcast_row(od_sb, 2048, sbB.t[:], sbB.tok, rowst)
            for g in range(16):
                st = stgr.next()
                dma(SP, st.t[:], od_sw[g], writes=[st.tok])
                ps = pt.next()
                op(PE, lambda: nc.tensor.transpose(ps.t[:, 0:128], st.t[:], ident), reads=[st.tok, cs.tok], writes=[ps.tok])
                op(DVE, lambda: nc.vector.tensor_copy(out=swT.t[:, g, :], in_=ps.t[:, 0:128]), reads=[ps.tok], writes=[swT.tok])
            dma(GP, rt.t[:], router.rearrange("(c p) e -> p c e", p=128), writes=[rt.tok])

            wring0 = Ring([sb(ph, f"w0_{i}", [128, KC, 512], BF16) for i in range(3)])
            for l in range(2):
                Wv = ada_w[l].rearrange("(kc p) n -> p kc n", p=128)
                for n4 in range(0, 6 * KC, 4):
                    wb = wring0.next()
                    dma(GP, wb.t[:], Wv[:, :, n4 * 128:(n4 + 4) * 128], writes=[wb.tok])
                    for b in range(4):
                        ni = n4 + b
                        ps = pm.next()
                        for kc in range(KC):
                            op(PE, lambda: nc.tensor.matmul(ps.t[:, 0:2], wb.t[:, kc, b * 128:(b + 1) * 128], scT.t[:, kc, :], start=(kc == 0), stop=(kc == KC - 1)),
                               reads=[wb.tok, scT.tok], writes=[ps.tok])
                        op(DVE, lambda: nc.vector.tensor_scalar(out=modv[l].t[:, ni, :], in0=ps.t[:, 0:2], scalar1=adab[l].t[:, ni:ni + 1], scalar2=None, op0=ALU.add),
                           reads=[ps.tok, adab[l].tok], writes=[modv[l].tok])
                for i in range(2):
                    for cnd in range(2):
                        op(DVE, lambda: nc.vector.scalar_tensor_tensor(
                            out=Amod[l][i].t[:, :, cnd], in0=modv[l].t[:, (3 * i + 1) * KC:(3 * i + 2) * KC, cnd], scalar=1.0,
                            in1=ng[:, (l * 2 + i) * KC:(l * 2 + i + 1) * KC], op0=ALU.add, op1=ALU.mult),
                            reads=[modv[l].tok, prm.tok], writes=[Amod[l][i].tok])
            S.barrier()
            if stop == 0:
                return

        def mod_shift(l, i, c, cnd):
            return modv[l].t[:, (3 * i) * KC + c, cnd:cnd + 1]

        def mod_gate(l, i, c, cnd):
            return modv[l].t[:, (3 * i + 2) * KC + c, cnd:cnd + 1]

        mod_toks = [modv[0].tok, modv[1].tok]

        def modulate(xcur, xtoks, hT, htoks, l, i, cnd, sqr, tmpr, rstd):
            ssp = pa.next()
            for c in range(KC):
                sq = sqr.next()
                op(ACT, lambda: nc.scalar.activation(out=sq.t[:], in_=xcur.t[:, c, :], func=AF.Square), reads=[xtoks[c]], writes=[sq.tok])
                op(PE, lambda: nc.tensor.matmul(ssp.t[:], onesb, sq.t[:], start=(c == 0), stop=(c == KC - 1)), reads=[sq.tok, cb.tok], writes=[ssp.tok])
            op(ACT, lambda: nc.scalar.activation(out=rstd.t[:], in_=ssp.t[:], func=AF.Sqrt, scale=1.0 / D, bias=epsc), reads=[ssp.tok, cs.tok], writes=[rstd.tok])
            op(DVE, lambda: nc.vector.reciprocal(out=rstd.t[:], in_=rstd.t[:]), reads=[rstd.tok], writes=[rstd.tok])
            for c in range(KC):
                tm = tmpr.next()
                op(DVE, lambda: nc.vector.tensor_tensor(out=tm.t[:], in0=xcur.t[:, c, :], in1=rstd.t[:], op=ALU.mult), reads=[xtoks[c], rstd.tok], writes=[tm.tok])
                op(ACT, lambda: nc.scalar.activation(out=hT.t[:, c, :], in_=tm.t[:], func=AF.Identity, scale=Amod[l][i].t[:, c, cnd:cnd + 1], bias=mod_shift(l, i, c, cnd)),
                   reads=[tm.tok, Amod[l][i].tok, mod_toks[l]], writes=[htoks[c]])

        def linear(Wv, k0, kn, col_list, rhs_of, ntok, evac, wring, pring=None):
            pring = pring or pm
            for n0 in col_list:
                wb = wring.next()
                dma(GP, wb.t[:, 0:kn, :], Wv.blk(k0, kn, n0), writes=[wb.tok])
                ps = pring.next()
                for kc in range(kn):
                    rap, rtoks = rhs_of(kc)
                    op(PE, lambda: nc.tensor.matmul(ps.t[:, 0:ntok], wb.t[:, kc, :], rap, start=(kc == 0), stop=(kc == kn - 1)),
                       reads=[wb.tok] + rtoks, writes=[ps.tok])
                evac(n0, ps)

        def toks(n):
            return [Tok() for _ in range(n)]

        with ExitStack() as ph:
            stg = Ring([sb(ph, f"pst{i}", [128, 16, 512], F32) for i in range(2)])
            obr = Ring([sb(ph, f"pob{i}", [128, 4, 32, 128], BF16) for i in range(2)])
            ci = 0
            for wbo in all_wb:
                Wv = wbo.src.rearrange("(kc p) n -> p kc n", p=128)
                N = wbo.nb * 128
                for sl in range(wbo.nsl):
                    k0 = sl * wbo.knsl
                    kn = min(wbo.knsl, wbo.kct - k0)
                    for n0 in range(0, N, 512):
                        w = min(512, N - n0)
                        nb4 = w // 128
                        o = obr.next()
                        for h0 in range(0, kn, 16):
                            hn = min(16, kn - h0)
                            st = stg.next()
                            dma(SP, st.t[:, 0:hn, 0:w], Wv[:, k0 + h0:k0 + h0 + hn, n0:n0 + w], writes=[st.tok])
                            for b in range(nb4):
                                if ci % 2 == 0:
                                    op(DVE, lambda: nc.vector.tensor_copy(out=o.t[:, b, h0:h0 + hn, :], in_=st.t[:, 0:hn, b * 128:(b + 1) * 128]), reads=[st.tok], writes=[o.tok])
                                else:
                                    op(ACT, lambda: nc.scalar.copy(out=o.t[:, b, h0:h0 + hn, :], in_=st.t[:, 0:hn, b * 128:(b + 1) * 128]), reads=[st.tok], writes=[o.tok])
                                ci += 1
                        dst = wbo.sc[sl, n0 // 128:n0 // 128 + nb4][:, :, 0:kn * 128].rearrange("b p f -> p b f")
                        dma(GP, dst, o.t[:, 0:nb4, 0:kn, :].rearrange("p b k c -> p b (k c)"), reads=[o.tok])
            S.barrier()
            if stop == 0.5:
                return

        with ExitStack() as ph:
            xcur = sb(ph, "xcur", [128, KC, 512], F32)
            xtk = toks(KC)
            hT = sb(ph, "hT", [128, KC, 512], BF16)
            htk = toks(KC)
            XH = D // 2
            xtokr = Ring([sb(ph, f"xtok{i}", [128, XH], F32) for i in range(2)])
            sqr = Ring([sb(ph, f"sq{i}", [128, 512], BF16) for i in range(2)])
            tmpr = Ring([sb(ph, f"tm{i}", [128, 512], F32) for i in range(3)])
            rstd = sb(ph, "rstd", [128, 512], F32)
            wring = Ring([sb(ph, f"w1_{i}", [128, KC, 128], BF16) for i in range(3)])
            ropet = sb(ph, "ropet", [128, 6, 512], F32)
            f32r = Ring([sb(ph, f"f32_{i}", [128, 512], F32) for i in range(3)])
            b16r = Ring([sb(ph, f"b16_{i}", [128, 512], BF16) for i in range(4)])
            vtok = sb(ph, "vtok", [128, 4, 512], BF16)
            vatok = sb(ph, "vatok", [128, 4, 512], BF16)
            va32 = sb(ph, "va32", [128, 4, 512], F32)
            ka32 = va32
            Wv_in = WB_in

            for t in range(NT):
                tok0 = t * 512
                cnd = 0 if t == 0 else 1
                is_s = t > 0
                src = xp if t == 0 else xs[(t - 1) * 512:t * 512, :]
                for sub in range(4):
                    for half in range(2):
                        xt_ = xtokr.next()
                        dma(SP, xt_.t[:], src[sub * 128:(sub + 1) * 128, half * XH:(half + 1) * XH], writes=[xt_.tok])
                        for c0 in range(half * (KC // 2), (half + 1) * (KC // 2), 4):
                            nn = min(4, (half + 1) * (KC // 2) - c0)
                            ps = pt.next()
                            for cc in range(nn):
                                lc = c0 + cc - half * (KC // 2)
                                op(PE, lambda: nc.tensor.transpose(ps.t[:, cc * 128:(cc + 1) * 128], xt_.t[:, lc * 128:(lc + 1) * 128], ident),
                                   reads=[xt_.tok, cs.tok], writes=[ps.tok])
                            dst = xcur.t[:, c0:c0 + nn, sub * 128:(sub + 1) * 128]
                            srcp = ps.t[:, 0:nn * 128].rearrange("p (c t) -> p c t", t=128)
                            if (c0 // 4) % 2 == 0:
                                op(ACT, lambda: nc.scalar.copy(out=dst, in_=srcp), reads=[ps.tok], writes=xtk[c0:c0 + nn])
                            else:
                                op(DVE, lambda: nc.vector.tensor_copy(out=dst, in_=srcp), reads=[ps.tok], writes=xtk[c0:c0 + nn])
                dma(SP, xT.rearrange("c p t -> p c t")[:, :, tok0:tok0 + 512], xcur.t[:], reads=xtk)
                if dbg is not None and dbg[0] == 10:
                    S.barrier()
                    return
                if is_s:
                    p0 = (t - 1) * 512
                    dma(SP, ropet.t[:], rope.rearrange("k p t -> p k t")[:, :, p0:p0 + 512], writes=[ropet.tok])
                modulate(xcur, xtk, hT, htk, 0, 0, cnd, sqr, tmpr, rstd)
                if dbg is not None and dbg[0] == 11:
                    S.barrier()
                    return

                def rhs_h(kc):
                    return hT.t[:, kc, :], [htk[kc]]

                def do_rope(x32, perm32, ci, si, out_ap, out_tok):
                    pr = pa.next()
                    op(PE, lambda: nc.tensor.matmul(pr.t[:], perm32, x32.t[:], start=True, stop=True), reads=[x32.tok, cs.tok], writes=[pr.tok])
                    t1 = f32r.next()
                    op(DVE, lambda: nc.vector.tensor_tensor(out=t1.t[:], in0=pr.t[:], in1=ropet.t[:, si, :], op=ALU.mult), reads=[pr.tok, ropet.tok], writes=[t1.tok])
                    op(DVE, lambda: nc.vector.tensor_tensor(out=x32.t[:], in0=x32.t[:], in1=ropet.t[:, ci, :], op=ALU.mult), reads=[x32.tok, ropet.tok], writes=[x32.tok])
                    op(DVE, lambda: nc.vector.tensor_tensor(out=out_ap, in0=x32.t[:], in1=t1.t[:], op=ALU.add), reads=[x32.tok, t1.tok], writes=[out_tok])

                def to_tokmajor(srcT, dst, col0, dt32):
                    ps = pt.next()
                    if dt32:
                        for sub in range(4):
                            op(PE, lambda: nc.tensor.transpose(ps.t[:, sub * 128:(sub + 1) * 128], srcT.t[:, sub * 128:(sub + 1) * 128], ident),
                               reads=[srcT.tok, cs.tok], writes=[ps.tok])
                        pv = ps.t[:, :].rearrange("p (s f) -> p s f", f=128)
                    else:
                        pb = ps.t[:, :].bitcast(BF16)
                        for sub in range(4):
                            op(PE, lambda: nc.tensor.transpose(pb[:, sub * 128:(sub + 1) * 128], srcT.t[:, sub * 128:(sub + 1) * 128], identb),
                               reads=[srcT.tok, cb.tok], writes=[ps.tok])
                        pv = pb[:, 0:512].rearrange("p (s f) -> p s f", f=128)
                    return ps, pv

                def evac_in(n0, ps):
                    n = n0 // 128
                    if n < 32:
                        dst_s = qr_s if n < 16 else kr_s
                        c = n % 16
                        ob = b16r.next()
                        if is_s:
                            x32 = f32r.next()
                            op(ACT, lambda: nc.scalar.copy(out=x32.t[:], in_=ps.t[:]), reads=[ps.tok], writes=[x32.tok])
                            ci = 0 if c % 2 == 0 else 2
                            do_rope(x32, permr32, ci, ci + 1, ob.t[:], ob.tok)
                        else:
                            op(ACT, lambda: nc.scalar.copy(out=ob.t[:], in_=ps.t[:]), reads=[ps.tok], writes=[ob.tok])
                        dma(SP, dst_s[c, :, tok0:tok0 + 512], ob.t[:], reads=[ob.tok])
                    elif n < 48:
                        c = n - 32
                        ob = b16r.next()
                        op(ACT, lambda: nc.scalar.copy(out=ob.t[:], in_=ps.t[:]), reads=[ps.tok], writes=[ob.tok])
                        p2, pv = to_tokmajor(ob, vtok, c * 128, False)
                        op(DVE, lambda: nc.vector.tensor_copy(out=vtok.t[:, :, (c % 4) * 128:(c % 4 + 1) * 128], in_=pv), reads=[p2.tok], writes=[vtok.tok])
                        if c % 4 == 3:
                            dma(SP, vr_s[tok0:tok0 + 512, (c - 3) * 128:(c + 1) * 128].rearrange("(s p) f -> p s f", p=128), vtok.t[:], reads=[vtok.tok])
                    elif n < 64:
                        c = n - 48
                        ob = b16r.next()
                        op(ACT, lambda: nc.scalar.activation(out=ob.t[:], in_=ps.t[:], func=AF.Silu), reads=[ps.tok], writes=[ob.tok])
                        dma(SP, gr_s[c, :, tok0:tok0 + 512], ob.t[:], reads=[ob.tok])
                    elif n < 84:
                        isq = n < 80
                        hh = n - 64 if isq else n - 80
                        gcol = qg if isq else kg
                        sq = sqr.next()
                        op(ACT, lambda: nc.scalar.activation(out=sq.t[:], in_=ps.t[:], func=AF.Square), reads=[ps.tok], writes=[sq.tok])
                        ssp = pa.next()
                        op(PE, lambda: nc.tensor.matmul(ssp.t[:], onesb, sq.t[:], start=True, stop=True), reads=[sq.tok, cb.tok], writes=[ssp.tok])
                        r32 = f32r.next()
                        op(ACT, lambda: nc.scalar.activation(out=r32.t[:], in_=ssp.t[:], func=AF.Sqrt, scale=1.0 / 128, bias=epsc), reads=[ssp.tok, cs.tok], writes=[r32.tok])
                        op(DVE, lambda: nc.vector.reciprocal(out=r32.t[:], in_=r32.t[:]), reads=[r32.tok], writes=[r32.tok])
                        x32 = f32r.next()
                        op(DVE, lambda: nc.vector.scalar_tensor_tensor(out=x32.t[:], in0=ps.t[:], scalar=gcol, in1=r32.t[:], op0=ALU.mult, op1=ALU.mult),
                           reads=[ps.tok, prm.tok, r32.tok], writes=[x32.tok])
                        ob = b16r.next()
                        if is_s:
                            do_rope(x32, perma32, 4, 5, ob.t[:], ob.tok)
                        else:
                            op(ACT, lambda: nc.scalar.copy(out=ob.t[:], in_=x32.t[:]), reads=[x32.tok], writes=[ob.tok])
                            if not isq:
                                p2, pv = to_tokmajor(x32, ka32, hh * 128, True)
                                op(DVE, lambda: nc.vector.tensor_copy(out=ka32.t[:, :, hh * 128:(hh + 1) * 128], in_=pv), reads=[p2.tok], writes=[ka32.tok])
                                if hh == 3:
                                    dma(SP, nk.rearrange("(s p) f -> p s f", p=128), ka32.t[:], reads=[ka32.tok])
                        dst_s = qa_s if isq else ka_s
                        dma(SP, dst_s[hh, :, tok0:tok0 + 512], ob.t[:], reads=[ob.tok])
                    else:
                        hh = n - 84
                        x32 = f32r.next()
                        op(ACT, lambda: nc.scalar.copy(out=x32.t[:], in_=ps.t[:]), reads=[ps.tok], writes=[x32.tok])
                        p2, pv = to_tokmajor(x32, va32, hh * 128, True)
                        op(DVE, lambda: nc.vector.tensor_copy(out=vatok.t[:, :, hh * 128:(hh + 1) * 128], in_=pv), reads=[p2.tok], writes=[vatok.tok])
                        if not is_s:
                            op(ACT, lambda: nc.scalar.copy(out=va32.t[:, :, hh * 128:(hh + 1) * 128], in_=pv), reads=[p2.tok], writes=[va32.tok])
                        if hh == 3:
                            dma(SP, va_s[tok0:tok0 + 512, :].rearrange("(s p) f -> p s f", p=128), vatok.t[:], reads=[vatok.tok])
                            if not is_s:
                                dma(SP, nv.rearrange("(s p) f -> p s f", p=128), va32.t[:], reads=[va32.tok])

                cl = [n * 128 for n in range(88)]
                if dbg is not None and dbg[0] == 12:
                    cl = [n * 128 for n in dbg[1]]
                linear(Wv_in, 0, KC, cl, rhs_h, 512, evac_in, wring)
                if dbg is not None and dbg[0] == 12 and t == dbg[2]:
                    S.barrier()
                    return
            S.barrier()
            if stop == 1:
                return

        seqs = [(0, 2, None), (256, 2, None), (512, DSEQ // 128, 0)]
        with ExitStack() as ph:
            DT = sb(ph, "DT", [128, 8, 128], F32)
            Qf = sb(ph, "Qf", [128, 8, 2, 128], F32)
            Qb = sb(ph, "Qb", [128, 8, 2, 128], F32)
            dec = sb(ph, "dec", [128, 512], F32)
            dma(SP, dec.t[:], cst[:, 128:640], writes=[dec.tok])
            MfT = dec.t[:, 0:128]
            MbT = dec.t[:, 128:256]
            IP1 = dec.t[:, 256:384]
            I128M = dec.t[:, 384:512]
            tmpa = sb(ph, "tmpa", [128, 128], F32)
            tmpb = sb(ph, "tmpb", [128, 128], F32)
            for h in range(8):
                op(DVE, lambda: nc.vector.tensor_scalar(out=tmpa.t[:], in0=MfT, scalar1=lg[:, h:h + 1], scalar2=None, op0=ALU.mult),
                   reads=[dec.tok, retc.tok], writes=[tmpa.tok])
                op(DVE, lambda: nc.vector.scalar_tensor_tensor(out=tmpb.t[:], in0=MbT, scalar=lg[:, 8 + h:9 + h], in1=tmpa.t[:], op0=ALU.mult, op1=ALU.add),
                   reads=[dec.tok, retc.tok, tmpa.tok], writes=[tmpb.tok])
                op(ACT, lambda: nc.scalar.activation(out=tmpa.t[:], in_=tmpb.t[:], func=AF.Exp), reads=[tmpb.tok], writes=[tmpa.tok])
                op(DVE, lambda: nc.vector.tensor_scalar(out=DT.t[:, h, :], in0=tmpa.t[:], scalar1=0.0625, scalar2=None, op0=ALU.mult),
                   reads=[tmpa.tok], writes=[DT.tok])
                for hf in range(2):
                    op(ACT, lambda: nc.scalar.activation(out=Qf.t[:, h, hf, :], in_=IP1, func=AF.Exp, scale=lg[:, h:h + 1]),
                       reads=[dec.tok, retc.tok], writes=[Qf.tok])
                    op(ACT, lambda: nc.scalar.activation(out=Qb.t[:, h, hf, :], in_=I128M, func=AF.Exp, scale=lg[:, 8 + h:9 + h]),
                       reads=[dec.tok, retc.tok], writes=[Qb.tok])
            Sf32 = sb(ph, "Sf32", [128, 8, 2, 256], F32)
            Sfb = sb(ph, "Sfb", [128, 8, 2, 256], BF16)
            sft = toks(8)
            sfbt = toks(8)
            kcr = Ring([sb(ph, f"kc{i}", [128, 16, 128], BF16) for i in range(2)])
            qcr = Ring([sb(ph, f"qc{i}", [128, 16, 128], BF16) for i in range(2)])
            gcr = Ring([sb(ph, f"gc{i}", [128, 16, 128], BF16) for i in range(2)])
            vcr = Ring([sb(ph, f"vc{i}", [128, 2048], BF16) for i in range(2)])
            snr = Ring([sb(ph, f"sn{i}", [128, 8, 2, 256], BF16) for i in range(2)])
            kdr = Ring([sb(ph, f"kd{i}", [128, 256], BF16) for i in range(3)])
            atr = Ring([sb(ph, f"at{i}", [128, 128], BF16) for i in range(3)])
            qfr = Ring([sb(ph, f"qf{i}", [128, 2, 128], BF16) for i in range(3)])
            qbr = Ring([sb(ph, f"qb{i}", [128, 2, 128], BF16) for i in range(3)])
            onr = Ring([sb(ph, f"on{i}", [128, 256], BF16) for i in range(3)])
            jkr = Ring([sb(ph, f"jk{i}", [128, 256], BF16) for i in range(2)])
            smr = Ring([sb(ph, f"sm{i}", [128, 4], F32) for i in range(4)])
            mixr = Ring([sb(ph, f"mx{i}", [128, 16, 128], BF16) for i in range(2)])

            def k_tokmajor(kc_, h, col):
                ps = pt.next()
                pb = ps.t[:, :].bitcast(BF16)
                for hf in range(2):
                    op(PE, lambda: nc.tensor.transpose(pb[:, hf * 128:(hf + 1) * 128], kc_.t[:, 2 * h + hf, :], identb), reads=[kc_.tok, cb.tok], writes=[ps.tok])
                kd = kdr.next()
                op(ACT, lambda: nc.scalar.activation(out=kd.t[:], in_=pb[:, 0:256], func=AF.Copy, scale=KD[:, col:col + 1]), reads=[ps.tok, retc.tok], writes=[kd.tok])
                return kd

            def state_update(kd, vc_, h, col):
                ps = pm.next()
                for hf in range(2):
                    op(PE, lambda: nc.tensor.matmul(ps.t[:, hf * 256:(hf + 1) * 256], kd.t[:, hf * 128:(hf + 1) * 128], vc_.t[:, h * 256:(h + 1) * 256], start=True, stop=True),
                       reads=[kd.tok, vc_.tok], writes=[ps.tok])
                sv = Sf32.t[:, h, :, :].rearrange("p a v -> p (a v)")
                op(DVE, lambda: nc.vector.scalar_tensor_tensor(out=sv, in0=sv, scalar=CD[:, col:col + 1], in1=ps.t[:], op0=ALU.mult, op1=ALU.add),
                   reads=[ps.tok, retc.tok, sft[h]], writes=[sft[h]])
                op(ACT, lambda: nc.scalar.copy(out=Sfb.t[:, h, :, :].rearrange("p a v -> p (a v)"), in_=sv), reads=[sft[h]], writes=[sfbt[h]])

            def init_state(src):
                if src is None:
                    op(DVE, lambda: nc.vector.memset(Sf32.t[:], 0.0), writes=sft)
                    op(DVE, lambda: nc.vector.memset(Sfb.t[:], 0.0), writes=sfbt)
                else:
                    for h in range(8):
                        dma(SP, Sf32.t[:, h, :, :], src[h].rearrange("(a p) v -> p a v", p=128), writes=[sft[h]])
                    op(ACT, lambda: nc.scalar.copy(out=Sfb.t[:], in_=Sf32.t[:]), reads=sft, writes=sfbt)

            for si, (stok0, nch, smp) in enumerate(seqs):
                init_state(None if smp is None else sb_in)
                for n in range(nch - 1, -1, -1):
                    c0 = stok0 + n * 128
                    kc_ = kcr.next()
                    vc_ = vcr.next()
                    dma(SP, kc_.t[:], kr_s.rearrange("c p t -> p c t")[:, :, c0:c0 + 128], writes=[kc_.tok])
                    dma(SP, vc_.t[:], vr_s[c0:c0 + 128, :], writes=[vc_.tok])
                    dma(SP, snap_s[c0 // 128], Sfb.t[:].rearrange("p h a v -> p (h a v)"), reads=sfbt)
                    for h in range(8):
                        kd = k_tokmajor(kc_, h, 8 + h)
                        state_update(kd, vc_, h, 8 + h)
                if smp is None:
                    for h in range(8):
                        dma(SP, nsb[si, h].rearrange("(a p) v -> p a v", p=128), Sf32.t[:, h, :, :], reads=[sft[h]])
                S.barrier()
                if stop == 2:
                    return
                init_state(None if smp is None else sf_in)
                for n in range(nch):
                    c0 = stok0 + n * 128
                    kc_, vc_, qc_, gc_, sn_ = kcr.next(), vcr.next(), qcr.next(), gcr.next(), snr.next()
                    dma(SP, kc_.t[:], kr_s.rearrange("c p t -> p c t")[:, :, c0:c0 + 128], writes=[kc_.tok])
                    dma(SP, qc_.t[:], qr_s.rearrange("c p t -> p c t")[:, :, c0:c0 + 128], writes=[qc_.tok])
                    dma(SP, vc_.t[:], vr_s[c0:c0 + 128, :], writes=[vc_.tok])
                    dma(SP, gc_.t[:], gr_s.rearrange("c p t -> p c t")[:, :, c0:c0 + 128], writes=[gc_.tok])
                    dma(SP, sn_.t[:].rearrange("p h a v -> p (h a v)"), snap_s[c0 // 128], writes=[sn_.tok])
                    mx = mixr.next()
                    for h in range(8):
                        kd = k_tokmajor(kc_, h, h)
                        pat = pa.next()
                        for hf in range(2):
                            op(PE, lambda: nc.tensor.matmul(pat.t[:, 0:128], kc_.t[:, 2 * h + hf, :], qc_.t[:, 2 * h + hf, :], start=(hf == 0), stop=(hf == 1)),
                               reads=[kc_.tok, qc_.tok], writes=[pat.tok])
                        at = atr.next()
                        op(DVE, lambda: nc.vector.tensor_tensor(out=at.t[:], in0=pat.t[:, 0:128], in1=DT.t[:, h, :], op=ALU.mult), reads=[pat.tok, DT.tok], writes=[at.tok])
                        qf_, qb_ = qfr.next(), qbr.next()
                        op(DVE, lambda: nc.vector.tensor_tensor(out=qf_.t[:], in0=qc_.t[:, 2 * h:2 * h + 2, :], in1=Qf.t[:, h, :, :], op=ALU.mult), reads=[qc_.tok, Qf.tok], writes=[qf_.tok])
                        op(DVE, lambda: nc.vector.tensor_tensor(out=qb_.t[:], in0=qc_.t[:, 2 * h:2 * h + 2, :], in1=Qb.t[:, h, :, :], op=ALU.mult), reads=[qc_.tok, Qb.tok], writes=[qb_.tok])
                        po = pm.next()
                        vh = vc_.t[:, h * 256:(h + 1) * 256]
                        op(PE, lambda: nc.tensor.matmul(po.t[:, 0:256], at.t[:], vh, start=True, stop=False), reads=[at.tok, vc_.tok], writes=[po.tok])
                        for hf in range(2):
                            op(PE, lambda: nc.tensor.matmul(po.t[:, 0:256], qf_.t[:, hf, :], Sfb.t[:, h, hf, :], start=False, stop=False), reads=[qf_.tok, sfbt[h]], writes=[po.tok])
                        for hf in range(2):
                            op(PE, lambda: nc.tensor.matmul(po.t[:, 0:256], qb_.t[:, hf, :], sn_.t[:, h, hf, :], start=False, stop=(hf == 1)), reads=[qb_.tok, sn_.tok], writes=[po.tok])
                        state_update(kd, vc_, h, h)
                        jk = jkr.next()
                        sm = smr.next()
                        op(ACT, lambda: nc.scalar.activation(out=jk.t[:], in_=po.t[:, 0:256], func=AF.Square, accum_out=sm.t[:, 0:1]), reads=[po.tok], writes=[jk.tok, sm.tok])
                        op(ACT, lambda: nc.scalar.activation(out=sm.t[:, 1:2], in_=sm.t[:, 0:1], func=AF.Sqrt, scale=1.0 / 256, bias=epsc), reads=[sm.tok, cs.tok], writes=[sm.tok])
                        op(DVE, lambda: nc.vector.reciprocal(out=sm.t[:, 2:3], in_=sm.t[:, 1:2]), reads=[sm.tok], writes=[sm.tok])
                        on = onr.next()
                        op(ACT, lambda: nc.scalar.activation(out=on.t[:], in_=po.t[:, 0:256], func=AF.Copy, scale=sm.t[:, 2:3]), reads=[po.tok, sm.tok], writes=[on.tok])
                        p2 = pt.next()
                        pb = p2.t[:, :].bitcast(BF16)
                        for hf in range(2):
                            op(PE, lambda: nc.tensor.transpose(pb[:, hf * 128:(hf + 1) * 128], on.t[:, hf * 128:(hf + 1) * 128], identb), reads=[on.tok, cb.tok], writes=[p2.tok])
                        op(DVE, lambda: nc.vector.tensor_tensor(out=mx.t[:, 2 * h:2 * h + 2, :], in0=pb[:, 0:256].rearrange("p (a t) -> p a t", t=128), in1=gc_.t[:, 2 * h:2 * h + 2, :], op=ALU.mult),
                           reads=[p2.tok, gc_.tok], writes=[mx.tok])
                    dma(SP, mix_s.rearrange("c p t -> p c t")[:, 0:16, c0:c0 + 128], mx.t[:], reads=[mx.tok])
                if smp is None:
                    for h in range(8):
                        dma(SP, nsf[si, h].rearrange("(a p) v -> p a v", p=128), Sf32.t[:, h, :, :], reads=[sft[h]])
                S.barrier()
                if stop == 3:
                    return

        with ExitStack() as ph:
            kctx = sb(ph, "kctx", [128, 4, PAST], BF16)
            vctx = sb(ph, "vctx", [128, PAST // 128, 4, 130], BF16)
            cstage = sb(ph, "cstage", [128, PAST // 128, 512], BF16)
            op(DVE, lambda: nc.vector.memset(vctx.t[:], 1.0), writes=[vctx.tok])
            dma(GP, cstage.t[:], ck.rearrange("(b p) f -> p b f", p=128), writes=[cstage.tok])
            for b in range(PAST // 128):
                for hk in range(4):
                    p2 = pt.next()
                    pb = p2.t[:, :].bitcast(BF16)
                    op(PE, lambda: nc.tensor.transpose(pb[:, 0:128], cstage.t[:, b, hk * 128:(hk + 1) * 128], identb), reads=[cstage.tok, cb.tok], writes=[p2.tok])
                    op(DVE, lambda: nc.vector.tensor_copy(out=kctx.t[:, hk, b * 128:(b + 1) * 128], in_=pb[:, 0:128]), reads=[p2.tok], writes=[kctx.tok])
            for b in range(PAST // 128):
                dma(GP, vctx.t[:, b, :, 0:128], cv[b * 128:(b + 1) * 128, :].rearrange("p (h d) -> p h d", d=128), writes=[vctx.tok])

            NBUF = 4
            kbr = [sb(ph, f"kb{i}", [128, 4, 128], BF16) for i in range(NBUF)]
            vbr = [sb(ph, f"vb{i}", [128, 4, 130], BF16) for i in range(NBUF)]
            for v_ in vbr:
                op(DVE, lambda: nc.vector.memset(v_.t[:], 1.0), writes=[v_.tok])
            qbr2 = Ring([sb(ph, f"qa{i}", [128, 16, 128], BF16) for i in range(2)])
            ptr = Ring([sb(ph, f"pT{i}", [128, 8, 512], BF16) for i in range(2)])
            oar = Ring([sb(ph, f"oa{i}", [128, 4, 128], BF16) for i in range(2)])
            denr = Ring([sb(ph, f"dn{i}", [128, 8], F32) for i in range(2)])
            mixr = Ring([sb(ph, f"mxa{i}", [128, 16, 128], BF16) for i in range(2)])
            nblk_ctx = PAST // 128

            def load_kv(blk_tok0, slot):
                dma(SP, kbr[slot].t[:], ka_s.rearrange("c p t -> p c t")[:, :, blk_tok0:blk_tok0 + 128], writes=[kbr[slot].tok])
                dma(SP, vbr[slot].t[:, :, 0:128], va_s[blk_tok0:blk_tok0 + 128, :].rearrange("p (h d) -> p h d", d=128), writes=[vbr[slot].tok])

            for si, (stok0, nch, smp) in enumerate(seqs):
                loaded = {}
                for i in range(nch):
                    c0 = stok0 + i * 128
                    if smp is None:
                        blks = [(j, None) for j in range(nch)]
                    else:
                        blks = []
                        if i > 0:
                            blks.append((i - 1, maskPb))
                        blks.append((i, None))
                        if i < nch - 1:
                            blks.append((i + 1, maskNb))
                    for j, _ in blks:
                        if j not in loaded:
                            slot = j % NBUF
                            load_kv(stok0 + j * 128, slot)
                            loaded[j] = slot
                    qa_ = qbr2.next()
                    dma(SP, qa_.t[:], qa_s.rearrange("c p t -> p c t")[:, :, c0:c0 + 128], writes=[qa_.tok])
                    mx = mixr.next()
                    for hk in range(4):
                        keyl = [(kbr[loaded[j]].t[:, hk, :], kbr[loaded[j]].tok, vbr[loaded[j]].t[:, hk, :], vbr[loaded[j]].tok, m) for j, m in blks]
                        if smp is not None:
                            keyl += [(kctx.t[:, hk, b * 128:(b + 1) * 128], kctx.tok, vctx.t[:, b, hk, :], vctx.tok, None) for b in range(nblk_ctx)]
                        pT = ptr.next()
                        qrhs = qa_.t[:, hk * 4:(hk + 1) * 4, :].rearrange("p g t -> p (g t)")
                        for kb, (kap, ktok, vap, vtok_, m) in enumerate(keyl):
                            ps = pm.next()
                            op(PE, lambda: nc.tensor.matmul(ps.t[:], kap, qrhs, start=True, stop=True), reads=[ktok, qa_.tok], writes=[ps.tok])
                            op(ACT, lambda: nc.scalar.activation(out=pT.t[:, kb, :], in_=ps.t[:], func=AF.Exp, scale=float(128 ** -0.5)), reads=[ps.tok], writes=[pT.tok])
                            if m is not None:
                                op(DVE, lambda: nc.vector.tensor_tensor(out=pT.t[:, kb, :], in0=pT.t[:, kb, :], in1=m, op=ALU.mult), reads=[pT.tok, cb.tok], writes=[pT.tok])
                        oa = oar.next()
                        dn = denr.next()
                        for g2 in range(2):
                            po = pm.next()
                            for gg in range(2):
                                g = g2 * 2 + gg
                                for kb, (kap, ktok, vap, vtok_, m) in enumerate(keyl):
                                    op(PE, lambda: nc.tensor.matmul(po.t[:, gg * 130:(gg + 1) * 130], pT.t[:, kb, g * 128:(g + 1) * 128], vap, start=(kb == 0), stop=(kb == len(keyl) - 1)),
                                       reads=[pT.tok, vtok_], writes=[po.tok])
                            pov = po.t[:, 0:260].rearrange("p (g d) -> p g d", d=130)
                            for gg in range(2):
                                g = g2 * 2 + gg
                                hh = hk * 4 + g
                                op(DVE, lambda: nc.vector.tensor_tensor(out=dn.t[:, g:g + 1], in0=pov[:, gg, 128:129], in1=esink[:, hh:hh + 1], op=ALU.add), reads=[po.tok, retc.tok], writes=[dn.tok])
                                op(DVE, lambda: nc.vector.reciprocal(out=dn.t[:, 4 + g:5 + g], in_=dn.t[:, g:g + 1]), reads=[dn.tok], writes=[dn.tok])
                                op(ACT, lambda: nc.scalar.activation(out=oa.t[:, g, :], in_=pov[:, gg, 0:128], func=AF.Copy, scale=dn.t[:, 4 + g:5 + g]), reads=[po.tok, dn.tok], writes=[oa.tok])
                        p2 = pt.next()
                        pb = p2.t[:, :].bitcast(BF16)
                        for g in range(4):
                            op(PE, lambda: nc.tensor.transpose(pb[:, g * 128:(g + 1) * 128], oa.t[:, g, :], identb), reads=[oa.tok, cb.tok], writes=[p2.tok])
                        op(DVE, lambda: nc.vector.tensor_copy(out=mx.t[:, hk * 4:(hk + 1) * 4, :], in_=pb[:, 0:512].rearrange("p (g t) -> p g t", t=128)), reads=[p2.tok], writes=[mx.tok])
                    dma(SP, mix_s.rearrange("c p t -> p c t")[:, 16:32, c0:c0 + 128], mx.t[:], reads=[mx.tok])
            S.barrier()
            if stop == 4:
                return

        with ExitStack() as ph:
            xcur = sb(ph, "xcur3", [128, KC, 512], F32)
            xtk = toks(KC)
            hT = sb(ph, "hT3", [128, KC, 512], BF16)
            htk = toks(KC)
            MC = max(KC, 32)
            vT = sb(ph, "vT3", [128, MC, 512], BF16)
            vtk = toks(MC)
            sqr = Ring([sb(ph, f"sq3_{i}", [128, 512], BF16) for i in range(2)])
            tmpr = Ring([sb(ph, f"tm3_{i}", [128, 512], F32) for i in range(3)])
            rstd = sb(ph, "rstd3", [128, 512], F32)
            mean = sb(ph, "mean3", [128, 512], F32)
            act, atk = vT, vtk
            wringA = Ring([sb(ph, f"wA_{i}", [128, max(KC, 32), 128], BF16) for i in range(4)])
            wringB = wringA
            Gt = sb(ph, "G3", [128, 512], F32)
            gl = sb(ph, "gl3", [128, 4, 40], F32)
            gT = sb(ph, "gT3", [8, 512], F32)
            sel = sb(ph, "sel3", [8, NE, 128], F32)
            b16r = Ring([sb(ph, f"b163_{i}", [128, 512], BF16) for i in range(2)])
            dma(SP, sel.t[:].rearrange("k e m -> k (e m)"), selc, writes=[sel.tok])

            Wv_out = WB_out
            Wv_w1 = WB_w1
            Wv_w3 = WB_w3
            Wv_w2 = WB_w2
            Wv_oin = WB_oin
            Wv_oout = WB_oout

            def resid_evac(l, i, cnd):
                def ev(n0, ps):
                    c = n0 // 128
                    op(DVE, lambda: nc.vector.scalar_tensor_tensor(out=xcur.t[:, c, :], in0=ps.t[:], scalar=mod_gate(l, i, c, cnd), in1=xcur.t[:, c, :], op0=ALU.mult, op1=ALU.add),
                       reads=[ps.tok, mod_toks[l], xtk[c]], writes=[xtk[c]])
                return ev

            def glu_slices(W1v, W3v, W2v, hid, l, cnd, gate_fn):
                nh = hid // 128
                SL = W2v.knsl
                for s0 in range(0, nh, SL):
                    ns = min(SL, nh - s0)
                    G = gate_fn() if gate_fn else None
                    for j in range(ns):
                        col = (s0 + j) * 128
                        w1b, w3b = wringA.next(), wringA.next()
                        dma(GP, w1b.t[:, 0:KC, :], W1v.blk(0, KC, col), writes=[w1b.tok])
                        dma(GP, w3b.t[:, 0:KC, :], W3v.blk(0, KC, col), writes=[w3b.tok])
                        p1, p3 = pm.next(), pm.next()
                        for kc in range(KC):
                            op(PE, lambda: nc.tensor.matmul(p1.t[:], w1b.t[:, kc, :], hT.t[:, kc, :], start=(kc == 0), stop=(kc == KC - 1)), reads=[w1b.tok, htk[kc]], writes=[p1.tok])
                        for kc in range(KC):
                            op(PE, lambda: nc.tensor.matmul(p3.t[:], w3b.t[:, kc, :], hT.t[:, kc, :], start=(kc == 0), stop=(kc == KC - 1)), reads=[w3b.tok, htk[kc]], writes=[p3.tok])
                        s1 = tmpr.next()
                        op(ACT, lambda: nc.scalar.activation(out=s1.t[:], in_=p1.t[:], func=AF.Silu), reads=[p1.tok], writes=[s1.tok])
                        if G is None:
                            op(DVE, lambda: nc.vector.tensor_tensor(out=act.t[:, j, :], in0=p3.t[:], in1=s1.t[:], op=ALU.mult), reads=[p3.tok, s1.tok], writes=[atk[j]])
                        else:
                            op(DVE, lambda: nc.vector.tensor_tensor(out=s1.t[:], in0=p3.t[:], in1=s1.t[:], op=ALU.mult), reads=[p3.tok, s1.tok], writes=[s1.tok])
                            op(DVE, lambda: nc.vector.tensor_tensor(out=act.t[:, j, :], in0=s1.t[:], in1=G.t[:], op=ALU.mult), reads=[s1.tok, G.tok], writes=[atk[j]])
                    linear(W2v, s0, ns, [n * 128 for n in range(KC)], lambda kc: (act.t[:, kc, :], [atk[kc]]), 512, resid_evac(l, 1, cnd), wringB)

            def gelu_tanh(ps, out_ap, out_tok, extra_mul=None, extra_tok=None):
                a = tmpr.next()
                op(ACT, lambda: nc.scalar.activation(out=a.t[:], in_=ps.t[:], func=AF.Square), reads=[ps.tok], writes=[a.tok])
                op(DVE, lambda: nc.vector.tensor_scalar(out=a.t[:], in0=a.t[:], scalar1=0.044715, scalar2=1.0, op0=ALU.mult, op1=ALU.add), reads=[a.tok], writes=[a.tok])
                op(DVE, lambda: nc.vector.tensor_tensor(out=a.t[:], in0=a.t[:], in1=ps.t[:], op=ALU.mult), reads=[a.tok, ps.tok], writes=[a.tok])
                op(ACT, lambda: nc.scalar.activation(out=a.t[:], in_=a.t[:], func=AF.Sigmoid, scale=1.5957691216057308), reads=[a.tok], writes=[a.tok])
                if extra_mul is None:
                    op(DVE, lambda: nc.vector.tensor_tensor(out=out_ap, in0=a.t[:], in1=ps.t[:], op=ALU.mult), reads=[a.tok, ps.tok], writes=[out_tok])
                else:
                    op(DVE, lambda: nc.vector.tensor_tensor(out=a.t[:], in0=a.t[:], in1=ps.t[:], op=ALU.mult), reads=[a.tok, ps.tok], writes=[a.tok])
                    op(DVE, lambda: nc.vector.tensor_tensor(out=out_ap, in0=a.t[:], in1=extra_mul, op=ALU.mult), reads=[a.tok, extra_tok], writes=[out_tok])

            for t in range(NT):
                tok0 = t * 512
                cnd = 0 if t == 0 else 1
                dma(SP, xcur.t[:], xT.rearrange("c p t -> p c t")[:, :, tok0:tok0 + 512], writes=xtk)
                dma(SP, vT.t[:], mix_s.rearrange("c p t -> p c t")[:, :, tok0:tok0 + 512], writes=vtk)
                linear(Wv_out, 0, 32, [n * 128 for n in range(KC)], lambda kc: (vT.t[:, kc, :], [vtk[kc]]), 512, resid_evac(0, 0, cnd), wringA)
                modulate(xcur, xtk, hT, htk, 0, 1, cnd, sqr, tmpr, rstd)
                glu_slices(Wv_w1, Wv_w3, Wv_w2, FFN, 0, cnd, None)
                modulate(xcur, xtk, hT, htk, 1, 0, cnd, sqr, tmpr, rstd)
                s1p, s2p = pa.next(), pa.next()

                def ev_v(n0, ps):
                    c = (n0 - D) // 128
                    gelu_tanh(ps, vT.t[:, c, :], vtk[c])
                    sq = sqr.next()
                    op(ACT, lambda: nc.scalar.activation(out=sq.t[:], in_=vT.t[:, c, :], func=AF.Square), reads=[vtk[c]], writes=[sq.tok])
                    op(PE, lambda: nc.tensor.matmul(s1p.t[:], onesb, vT.t[:, c, :], start=(c == 0), stop=(c == KC - 1)), reads=[vtk[c], cb.tok], writes=[s1p.tok])
                    op(PE, lambda: nc.tensor.matmul(s2p.t[:], onesb, sq.t[:], start=(c == 0), stop=(c == KC - 1)), reads=[sq.tok, cb.tok], writes=[s2p.tok])

                linear(Wv_oin, 0, KC, [D + n * 128 for n in range(KC)], lambda kc: (hT.t[:, kc, :], [htk[kc]]), 512, ev_v, wringA)
                op(ACT, lambda: nc.scalar.activation(out=mean.t[:], in_=s1p.t[:], func=AF.Copy, scale=1.0 / D), reads=[s1p.tok], writes=[mean.tok])
                m2 = tmpr.next()
                op(DVE, lambda: nc.vector.tensor_tensor(out=m2.t[:], in0=mean.t[:], in1=mean.t[:], op=ALU.mult), reads=[mean.tok], writes=[m2.tok])
                op(DVE, lambda: nc.vector.scalar_tensor_tensor(out=rstd.t[:], in0=s2p.t[:], scalar=1.0 / D, in1=m2.t[:], op0=ALU.mult, op1=ALU.subtract), reads=[s2p.tok, m2.tok], writes=[rstd.tok])
                op(ACT, lambda: nc.scalar.activation(out=rstd.t[:], in_=rstd.t[:], func=AF.Sqrt, bias=epsc), reads=[rstd.tok, cs.tok], writes=[rstd.tok])
                op(DVE, lambda: nc.vector.reciprocal(out=rstd.t[:], in_=rstd.t[:]), reads=[rstd.tok], writes=[rstd.tok])
                for c in range(KC):
                    g = (c * 128) // (D // 16)
                    a = tmpr.next()
                    op(DVE, lambda: nc.vector.tensor_tensor(out=a.t[:], in0=vT.t[:, c, :], in1=mean.t[:], op=ALU.subtract), reads=[vtk[c], mean.tok], writes=[a.tok])
                    vn = b16r.next()
                    op(DVE, lambda: nc.vector.scalar_tensor_tensor(out=vn.t[:], in0=a.t[:], scalar=vg[:, c:c + 1], in1=rstd.t[:], op0=ALU.mult, op1=ALU.mult), reads=[a.tok, prm.tok, rstd.tok], writes=[vn.tok])
                    p2 = pt.next()
                    pb = p2.t[:, :].bitcast(BF16)
                    for sub in range(4):
                        op(PE, lambda: nc.tensor.transpose(pb[:, sub * 128:(sub + 1) * 128], vn.t[:, sub * 128:(sub + 1) * 128], identb), reads=[vn.tok, cb.tok], writes=[p2.tok])
                    vtm = b16r.next()
                    op(ACT, lambda: nc.scalar.copy(out=vtm.t[:], in_=pb[:, 0:512]), reads=[p2.tok], writes=[vtm.tok])
                    pmx = pm.next()
                    for sub in range(4):
                        op(PE, lambda: nc.tensor.matmul(pmx.t[:, sub * 128:(sub + 1) * 128], vtm.t[:, sub * 128:(sub + 1) * 128], swT.t[:, g, :], start=True, stop=True), reads=[vtm.tok, swT.tok], writes=[pmx.tok])
                    for sub in range(4):
                        op(DVE, lambda: nc.vector.tensor_tensor(out=vT.t[:, c, sub * 128:(sub + 1) * 128], in0=pmx.t[:, sub * 128:(sub + 1) * 128], in1=sbB.t[:, g * 128:(g + 1) * 128], op=ALU.add),
                           reads=[pmx.tok, sbB.tok], writes=[vtk[c]])

                def ev_u(n0, ps):
                    c = n0 // 128
                    gelu_tanh(ps, vT.t[:, c, :], vtk[c], extra_mul=vT.t[:, c, :], extra_tok=vtk[c])

                linear(Wv_oin, 0, KC, [n * 128 for n in range(KC)], lambda kc: (hT.t[:, kc, :], [htk[kc]]), 512, ev_u, wringA)
                linear(Wv_oout, 0, KC, [n * 128 for n in range(KC)], lambda kc: (vT.t[:, kc, :], [vtk[kc]]), 512, resid_evac(1, 0, cnd), wringA)
                modulate(xcur, xtk, hT, htk, 1, 1, cnd, sqr, tmpr, rstd)
                pg = pa.next()
                for sub in range(4):
                    pl = pm.next()
                    for kc in range(KC):
                        op(PE, lambda: nc.tensor.matmul(pl.t[:, 0:NE], hT.t[:, kc, sub * 128:(sub + 1) * 128], rt.t[:, kc, :], start=(kc == 0), stop=(kc == KC - 1)), reads=[htk[kc], rt.tok], writes=[pl.tok])
                    L = gl.t[:, sub, 0:8]
                    srt = gl.t[:, sub, 8:16]
                    E = gl.t[:, sub, 16:24]
                    nm = gl.t[:, sub, 24:25]
                    dn = gl.t[:, sub, 25:26]
                    Gs = gl.t[:, sub, 32:40]
                    op(DVE, lambda: nc.vector.tensor_copy(out=L, in_=pl.t[:, 0:NE]), reads=[pl.tok], writes=[gl.tok])
                    op(DVE, lambda: nc.vector.max(out=srt, in_=L), reads=[gl.tok], writes=[gl.tok])
                    op(DVE, lambda: nc.vector.tensor_scalar(out=nm, in0=srt[:, 0:1], scalar1=-1.0, scalar2=None, op0=ALU.mult), reads=[gl.tok], writes=[gl.tok])
                    op(ACT, lambda: nc.scalar.activation(out=E, in_=L, func=AF.Exp, bias=nm), reads=[gl.tok], writes=[gl.tok])
                    op(DVE, lambda: nc.vector.scalar_tensor_tensor(out=E, in0=L, scalar=srt[:, 1:2], in1=E, op0=ALU.is_ge, op1=ALU.mult), reads=[gl.tok], writes=[gl.tok])
                    op(DVE, lambda: nc.vector.tensor_reduce(out=dn, in_=E, axis=mybir.AxisListType.X, op=ALU.add), reads=[gl.tok], writes=[gl.tok])
                    op(DVE, lambda: nc.vector.reciprocal(out=dn, in_=dn), reads=[gl.tok], writes=[gl.tok])
                    op(DVE, lambda: nc.vector.tensor_scalar(out=Gs, in0=E, scalar1=dn, scalar2=None, op0=ALU.mult), reads=[gl.tok], writes=[gl.tok])
                    op(PE, lambda: nc.tensor.transpose(pg.t[0:8, sub * 128:(sub + 1) * 128], Gs, ident), reads=[gl.tok, cs.tok], writes=[pg.tok])
                op(DVE, lambda: nc.vector.tensor_copy(out=gT.t[:], in_=pg.t[0:8, :]), reads=[pg.tok], writes=[gT.tok])
                for e in range(NE):
                    def gate_fn(e=e):
                        pgb = pa.next()
                        op(PE, lambda: nc.tensor.matmul(pgb.t[:], sel.t[:, e, :], gT.t[:], start=True, stop=True), reads=[sel.tok, gT.tok], writes=[pgb.tok])
                        op(ACT, lambda: nc.scalar.copy(out=Gt.t[:], in_=pgb.t[:]), reads=[pgb.tok], writes=[Gt.tok])
                        return Gt
                    glu_slices(WB_m1[e], WB_m3[e], WB_m2[e], EXPD, 1, cnd, gate_fn)
                dst = yp if t == 0 else ys[(t - 1) * 512:t * 512, :]
                for c in range(KC):
                    yt = tmpr.next()
                    ps = pt.next()
                    for sub in range(4):
                        op(PE, lambda: nc.tensor.transpose(ps.t[:, sub * 128:(sub + 1) * 128], xcur.t[:, c, sub * 128:(sub + 1) * 128], ident), reads=[xtk[c], cs.tok], writes=[ps.tok])
                    if c % 2 == 0:
                        op(ACT, lambda: nc.scalar.copy(out=yt.t[:], in_=ps.t[:]), reads=[ps.tok], writes=[yt.tok])
                    else:
                        op(DVE, lambda: nc.vector.tensor_copy(out=yt.t[:], in_=ps.t[:]), reads=[ps.tok], writes=[yt.tok])
                    dma(SP, dst[:, c * 128:(c + 1) * 128].rearrange("(s p) f -> p s f", p=128), yt.t[:, :].rearrange("p (s f) -> p s f", f=128), reads=[yt.tok])
            S.barrier()
            if stop == 5:
                return
    with es:
        try:
            _body()
        except _Stop:
            pass
    return nc


_CFG = Cfg()
_NC_CACHE = {}


def make_in_maps(cfg, inputs, ncores):
    f = lambda a: np.ascontiguousarray(np.asarray(a, dtype=np.float32))
    hc = host_consts(cfg)
    D = cfg.D
    shared = {
        "norm_g": f(inputs["norm_g"]).reshape(4, D),
        "ada_w": f(inputs["ada_w"]),
        "ada_b": f(inputs["ada_b"]),
        "ev_w_in": f(inputs["ev_w_in"])[0],
        "ev_w_out": f(inputs["ev_w_out"])[0],
        "qkn": np.stack([f(inputs["ev_q_norm"])[0], f(inputs["ev_k_norm"])[0]], axis=0),
        "ev_sink": f(inputs["ev_sink"]).reshape(1, 16),
        "ev_ret_decay": f(inputs["ev_ret_decay"]).reshape(1, 16),
        "ffn_w1": f(inputs["ffn_w1"])[0],
        "ffn_w3": f(inputs["ffn_w3"])[0],
        "ffn_w2": f(inputs["ffn_w2"])[0],
        "od_w_in": f(inputs["od_w_in"])[0],
        "od_v_norm": f(inputs["od_v_norm"]).reshape(1, D),
        "od_spatial_w": f(inputs["od_spatial_w"])[0],
        "od_spatial_b": f(inputs["od_spatial_b"]).reshape(1, 16 * 128),
        "od_w_out": f(inputs["od_w_out"])[0],
        "moe_router": f(inputs["moe_router"])[0],
        "moe_w1": f(inputs["moe_w1"])[0],
        "moe_w3": f(inputs["moe_w3"])[0],
        "moe_w2": f(inputs["moe_w2"])[0],
        "cst": hc["cst"],
        "selc": hc["selc"],
        "rope": hc["rope"],
    }
    xp = f(inputs["x_prompt"])
    xs = f(inputs["x_sample"])
    ck = f(inputs["cache_attn_k"])
    cv = f(inputs["cache_attn_v"])
    sf = f(inputs["state_ret_fwd"])
    sbw = f(inputs["state_ret_bwd"])
    c = f(inputs["c"])
    cctx = f(inputs["c_ctx"])
    maps = []
    for i in range(ncores):
        m = dict(shared)
        m["xp"] = xp[2 * i:2 * i + 2].reshape(512, D)
        m["xs"] = xs[i]
        m["ck"] = ck[i, 0].reshape(cfg.PAST, 512)
        m["cv"] = cv[i, 0].reshape(cfg.PAST, 512)
        m["sf"] = sf[i, 0]
        m["sb"] = sbw[i, 0]
        m["cvec"] = np.stack([cctx, c[i]], axis=0)
        maps.append(m)
    return maps


def gather(cfg, results, ncores):
    D = cfg.D
    yp = np.stack([r["yp"].reshape(2, cfg.SEQ, D) for r in results]).reshape(2 * ncores, cfg.SEQ, D)
    ys = np.stack([r["ys"] for r in results])
    nk = np.stack([r["nk"].reshape(2, cfg.SEQ, 4, 128) for r in results]).reshape(2 * ncores, 1, cfg.SEQ, 4, 128)
    nv = np.stack([r["nv"].reshape(2, cfg.SEQ, 4, 128) for r in results]).reshape(2 * ncores, 1, cfg.SEQ, 4, 128)
    nsf = np.stack([r["nsf"] for r in results]).reshape(2 * ncores, 1, 8, 256, 256)
    nsb = np.stack([r["nsb"] for r in results]).reshape(2 * ncores, 1, 8, 256, 256)
    return tuple(np.ascontiguousarray(a.astype(np.float32)) for a in (yp, ys, nk, nv, nsf, nsb))


def kernel(**inputs):
    cfg = _CFG
    if "nc" not in _NC_CACHE:
        _NC_CACHE["nc"] = build(cfg)
    nc = _NC_CACHE["nc"]
    maps = make_in_maps(cfg, inputs, 8)
    res = run_bass_kernel_spmd(nc, maps, core_ids=list(range(8)))
    return gather(cfg, res.results, 8)
```
